# Optimizing a Trainium2 kernel written in Bass

```python
import jax, jax.numpy as jnp
from jax import lax
import numpy as np

D_MODEL = 1024
BATCH = 2
SEQ = 8192
DEPTH = 1

GRID_W = 64
CTX_LEN = 256
MLA_HEADS = 8
QK_NOPE = 128
QK_ROPE = 64
V_DIM = 128
Q_LORA = 384
KV_LORA = 256
ROPE_BASE = 10000.0
ROPE_PAIRS = QK_ROPE // 4
Q_BLOCK = 128
ATTN_SCALE = (QK_NOPE + QK_ROPE) ** -0.5
MLA_WIDTH = MLA_HEADS * V_DIM
ML_HEADS = 4
ML_INNER = 1024
ML_HEAD_DIM = ML_INNER // ML_HEADS
QKV_BLOCK = 4
QKV_NBLK = ML_INNER // QKV_BLOCK
CONV_W = 5
CHUNK = 128
N_EXPERTS = 16
EXPERT_FF = 1024
CAP_FACTOR = 2
EPS = 1e-6
IN_SIZES = (Q_LORA, KV_LORA, QK_ROPE, ML_INNER, ML_INNER, D_MODEL, D_MODEL)
IN_DIM = Q_LORA + KV_LORA + QK_ROPE + 2 * ML_INNER + 2 * D_MODEL

kernel_name = "hybrid_mla_mlstm_ec_moe_dit"


def rmsnorm(x, g):
    xf = x.astype(jnp.float32)
    y = xf * lax.rsqrt(jnp.mean(xf * xf, axis=-1, keepdims=True) + EPS)
    return (y * g.astype(jnp.float32)).astype(x.dtype)


def modulate(h, shift, scale):
    return h * (1 + scale[:, None, :]) + shift[:, None, :]


def axial_angles(T):
    rows = T // GRID_W
    row = jnp.repeat(jnp.arange(rows, dtype=jnp.float32), GRID_W)
    col = jnp.tile(jnp.arange(GRID_W, dtype=jnp.float32), rows)
    inv = ROPE_BASE ** (-jnp.arange(ROPE_PAIRS, dtype=jnp.float32) / ROPE_PAIRS)
    return row[:, None] * inv, col[:, None] * inv


def rotate(x, ang):
    cos = jnp.cos(ang)[None, :, None, :]
    sin = jnp.sin(ang)[None, :, None, :]
    xf = x.astype(jnp.float32)
    x1, x2 = xf[..., :ROPE_PAIRS], xf[..., ROPE_PAIRS:]
    return jnp.concatenate([x1 * cos - x2 * sin, x2 * cos + x1 * sin], -1).astype(x.dtype)


def rope_2d(x, ang_row, ang_col):
    half = QK_ROPE // 2
    return jnp.concatenate([rotate(x[..., :half], ang_row), rotate(x[..., half:], ang_col)], -1)


def mla_qkv(q_lat, kv_lat, k_rope, q_norm, w_uq, kv_norm, w_ukv, angles):
    B, T, _ = q_lat.shape
    q = (rmsnorm(q_lat, q_norm) @ w_uq).reshape(B, T, MLA_HEADS, QK_NOPE + QK_ROPE)
    kv = (rmsnorm(kv_lat, kv_norm) @ w_ukv).reshape(B, T, MLA_HEADS, QK_NOPE + V_DIM)
    q_nope, q_pe = q[..., :QK_NOPE], q[..., QK_NOPE:]
    k_nope, v = kv[..., :QK_NOPE], kv[..., QK_NOPE:]
    k_pe = k_rope[:, :, None, :]
    if angles is not None:
        q_pe = rope_2d(q_pe, *angles)
        k_pe = rope_2d(k_pe, *angles)
    k_pe = jnp.broadcast_to(k_pe, (B, T, MLA_HEADS, QK_ROPE))
    return (jnp.concatenate([q_nope, q_pe], -1), jnp.concatenate([k_nope, k_pe], -1), v)


def attend(q, k, v):
    B, Tq, H, Dk = q.shape
    nb = Tq // Q_BLOCK
    qb = q.reshape(B, nb, Q_BLOCK, H, Dk).transpose(1, 0, 2, 3, 4)

    def one_block(qi):
        s = jnp.einsum('bqhd,bkhd->bhqk', qi, k).astype(jnp.float32) * ATTN_SCALE
        p = jax.nn.softmax(s, axis=-1).astype(v.dtype)
        return jnp.einsum('bhqk,bkhd->bqhd', p, v)

    o = lax.map(one_block, qb)
    return o.transpose(1, 0, 2, 3, 4).reshape(B, Tq, H * v.shape[-1])


def dwconv(x, w, b):
    out = lax.conv_general_dilated(x, w[:, None, :].astype(x.dtype), (1,), [(CONV_W // 2, CONV_W // 2)],
                                   dimension_numbers=('NWC', 'WIO', 'NWC'), feature_group_count=x.shape[-1])
    return out + b


def blockdiag(x, w):
    B, T, _ = x.shape
    y = jnp.einsum('btnj,njk->btnk', x.reshape(B, T, QKV_NBLK, QKV_BLOCK), w)
    return y.reshape(B, T, ML_INNER)


def mlstm_features(x_m, conv_w, conv_b, w_qblk, w_kblk, w_vblk, w_gate, b_gate):
    B, T, _ = x_m.shape
    x_c = jax.nn.silu(dwconv(x_m, conv_w, conv_b))
    q = blockdiag(x_c, w_qblk)
    k = blockdiag(x_c, w_kblk)
    v = blockdiag(x_m, w_vblk)
    gates = (jnp.concatenate([q, k, v], -1) @ w_gate + b_gate).reshape(B, T, 2, 2, ML_HEADS)
    gates = gates.astype(jnp.float32).transpose(2, 3, 0, 4, 1)
    heads = lambda a: a.reshape(B, T, ML_HEADS, ML_HEAD_DIM).transpose(0, 2, 1, 3).astype(jnp.float32)
    return heads(q), heads(k) * (ML_HEAD_DIM ** -0.5), heads(v), gates, x_c


def mlstm_chunkwise(q, k, v, ig, lf, state):
    B, H, T, dh = q.shape
    nc = T // CHUNK
    ch = lambda a: jnp.moveaxis(a.reshape(B, H, nc, CHUNK, *a.shape[3:]), 2, 0)
    mask = jnp.tril(jnp.ones((CHUNK, CHUNK), dtype=bool))

    def step(carry, xs):
        C, n, m = carry
        qc, kc, vc, ic, fc = xs
        b = jnp.cumsum(fc, axis=-1)
        dmat = jnp.where(mask, b[..., :, None] - b[..., None, :] + ic[..., None, :], -jnp.inf)
        inter = b + m[..., None]
        m_t = jnp.maximum(inter, jnp.max(dmat, axis=-1))
        w_inter = jnp.exp(inter - m_t)
        s = jnp.einsum('bhtd,bhsd->bhts', qc, kc) * jnp.exp(dmat - m_t[..., None])
        num = jnp.einsum('bhts,bhsd->bhtd', s, vc) + w_inter[..., None] * jnp.einsum('bhvd,bhtd->bhtv', C, qc)
        den = jnp.sum(s, axis=-1) + w_inter * jnp.einsum('bhd,bhtd->bht', n, qc)
        h = num / jnp.maximum(jnp.abs(den), jnp.exp(-m_t))[..., None]
        b_last = b[..., -1]
        dec = b_last[..., None] - b + ic
        m_new = jnp.maximum(b_last + m, jnp.max(dec, axis=-1))
        wk = jnp.exp(dec - m_new[..., None])
        keep = jnp.exp(b_last + m - m_new)
        C_new = keep[..., None, None] * C + jnp.einsum('bhsv,bhsd->bhvd', vc * wk[..., None], kc)
        n_new = keep[..., None] * n + jnp.einsum('bhs,bhsd->bhd', wk, kc)
        return (C_new, n_new, m_new), h

    state, hs = lax.scan(step, state, (ch(q), ch(k), ch(v), ch(ig), ch(lf)))
    return jnp.moveaxis(hs, 0, 2).reshape(B, H, T, dh), state


def mlstm_bidir(feat_c, feat_l):
    qc, kc, vc, gc = feat_c
    ql, kl, vl, gl = feat_l
    B, H, _, dh = qc.shape
    zero = (jnp.zeros((B, H, dh, dh), jnp.float32), jnp.zeros((B, H, dh), jnp.float32),
            jnp.zeros((B, H), jnp.float32))
    lsig = jax.nn.log_sigmoid
    flip = lambda a: jnp.flip(a, axis=2)
    hc_f, st_f = mlstm_chunkwise(qc, kc, vc, gc[0, 0], lsig(gc[0, 1]), zero)
    hl_f, _ = mlstm_chunkwise(ql, kl, vl, gl[0, 0], lsig(gl[0, 1]), st_f)
    hc_b, st_b = mlstm_chunkwise(flip(qc), flip(kc), flip(vc), flip(gc[1, 0]), lsig(flip(gc[1, 1])), zero)
    hl_b, _ = mlstm_chunkwise(flip(ql), flip(kl), flip(vl), flip(gl[1, 0]), lsig(flip(gl[1, 1])), st_b)
    return hc_f + flip(hc_b), hl_f + flip(hl_b)


def mlstm_readout(h, z, x_c, ml_norm, ml_skip):
    B, H, T, dh = h.shape
    hf = h.transpose(0, 2, 1, 3)
    hn = hf * lax.rsqrt(jnp.mean(hf * hf, axis=-1, keepdims=True) + EPS)
    hn = hn.reshape(B, T, ML_INNER).astype(z.dtype) * ml_norm
    return jax.nn.sigmoid(z) * (hn + ml_skip * x_c)


def merge(y_mla, y_ml, g_mla, g_ml, w_out):
    return (jax.nn.sigmoid(g_mla) * y_mla + jax.nn.sigmoid(g_ml) * y_ml) @ w_out


def mixer(h_c, h_l, angles, last, w_in, q_norm, w_uq, kv_norm, w_ukv, conv_w, conv_b,
          w_qblk, w_kblk, w_vblk, w_gate, b_gate, ml_norm, ml_skip, w_out):
    splits = [int(s) for s in np.cumsum(IN_SIZES)[:-1]]
    qlat_c, kvlat_c, krope_c, xm_c, z_c, gmla_c, gml_c = jnp.split(h_c @ w_in, splits, axis=-1)
    qlat_l, kvlat_l, krope_l, xm_l, z_l, gmla_l, gml_l = jnp.split(h_l @ w_in, splits, axis=-1)
    q_c, k_c, v_c = mla_qkv(qlat_c, kvlat_c, krope_c, q_norm, w_uq, kv_norm, w_ukv, None)
    q_l, k_l, v_l = mla_qkv(qlat_l, kvlat_l, krope_l, q_norm, w_uq, kv_norm, w_ukv, angles)
    y_mla_l = attend(q_l, jnp.concatenate([k_c, k_l], 1), jnp.concatenate([v_c, v_l], 1))
    fc = mlstm_features(xm_c, conv_w, conv_b, w_qblk, w_kblk, w_vblk, w_gate, b_gate)
    fl = mlstm_features(xm_l, conv_w, conv_b, w_qblk, w_kblk, w_vblk, w_gate, b_gate)
    hc, hl = mlstm_bidir(fc[:4], fl[:4])
    out_l = merge(y_mla_l, mlstm_readout(hl, z_l, fl[4], ml_norm, ml_skip), gmla_l, gml_l, w_out)
    if last:
        return None, out_l
    y_mla_c = attend(q_c, k_c, v_c)
    out_c = merge(y_mla_c, mlstm_readout(hc, z_c, fc[4], ml_norm, ml_skip), gmla_c, gml_c, w_out)
    return out_c, out_l


def ec_moe(x, w_router, w_e_gate, w_e_up, w_e_down):
    B, T, D = x.shape
    cap = CAP_FACTOR * T // N_EXPERTS
    aff = jax.nn.softmax((x @ w_router).astype(jnp.float32), axis=-1)
    g, idx = lax.top_k(aff.transpose(0, 2, 1), cap)
    xs = jax.vmap(lambda xb, ib: xb[ib])(x, idx)
    a = jnp.einsum('becd,edf->becf', xs, w_e_gate)
    u = jnp.einsum('becd,edf->becf', xs, w_e_up)
    y = jnp.einsum('becf,efd->becd', jax.nn.silu(a) * u, w_e_down) * g[..., None].astype(x.dtype)
    return jax.vmap(lambda yb, ib: jnp.zeros((T, D), x.dtype).at[ib.reshape(-1)].add(yb.reshape(-1, D)))(y, idx)


def setup_inputs(seed: int = 0) -> dict:
    key = jax.random.key(seed)
    ks = jax.random.split(key, 32)
    f32 = jnp.float32
    nrm = lambda k, shape, fan: jax.random.normal(k, shape, f32) * (fan ** -0.5)
    gain = lambda k, shape: 1.0 + 0.02 * jax.random.normal(k, shape, f32)
    L, D = DEPTH, D_MODEL
    b_gate = (0.1 * jax.random.normal(ks[16], (L, 2, 2, ML_HEADS), f32)
              + jnp.array([0.0, 3.0], f32)[None, None, :, None]).reshape(L, 4 * ML_HEADS)
    return {
        "x": jax.random.normal(ks[0], (BATCH, SEQ, D), f32),
        "c": jax.random.normal(ks[1], (BATCH, D), f32),
        "ctx": jax.random.normal(ks[2], (BATCH, CTX_LEN, D), f32),
        "c_ctx": jax.random.normal(ks[3], (D,), f32),
        "w_mod": nrm(ks[4], (L, D, 6 * D), D) * 0.5,
        "b_mod": 0.02 * jax.random.normal(ks[5], (L, 6 * D), f32),
        "norm1": gain(ks[6], (L, D)),
        "w_in": nrm(ks[7], (L, D, IN_DIM), D),
        "q_norm": gain(ks[8], (L, Q_LORA)),
        "w_uq": nrm(ks[9], (L, Q_LORA, MLA_HEADS * (QK_NOPE + QK_ROPE)), Q_LORA),
        "kv_norm": gain(ks[10], (L, KV_LORA)),
        "w_ukv": nrm(ks[11], (L, KV_LORA, MLA_HEADS * (QK_NOPE + V_DIM)), KV_LORA),
        "conv_w": nrm(ks[12], (L, CONV_W, ML_INNER), CONV_W),
        "conv_b": 0.02 * jax.random.normal(ks[13], (L, ML_INNER), f32),
        "w_qblk": nrm(ks[14], (L, QKV_NBLK, QKV_BLOCK, QKV_BLOCK), QKV_BLOCK),
        "w_kblk": nrm(ks[15], (L, QKV_NBLK, QKV_BLOCK, QKV_BLOCK), QKV_BLOCK),
        "w_vblk": nrm(ks[17], (L, QKV_NBLK, QKV_BLOCK, QKV_BLOCK), QKV_BLOCK),
        "w_gate": nrm(ks[18], (L, 3 * ML_INNER, 4 * ML_HEADS), 3 * ML_INNER) * 0.5,
        "b_gate": b_gate,
        "ml_norm": gain(ks[19], (L, ML_INNER)),
        "ml_skip": gain(ks[20], (L, ML_INNER)),
        "w_out": nrm(ks[21], (L, D, D), D),
        "norm2": gain(ks[22], (L, D)),
        "w_router": nrm(ks[23], (L, D, N_EXPERTS), D),
        "w_e_gate": nrm(ks[24], (L, N_EXPERTS, D, EXPERT_FF), D),
        "w_e_up": nrm(ks[25], (L, N_EXPERTS, D, EXPERT_FF), D),
        "w_e_down": nrm(ks[26], (L, N_EXPERTS, EXPERT_FF, D), EXPERT_FF),
        "final_norm": gain(ks[27], (D,)),
    }


def reference(x, c, ctx, c_ctx, w_mod, b_mod, norm1, w_in, q_norm, w_uq, kv_norm, w_ukv, conv_w, conv_b,
              w_qblk, w_kblk, w_vblk, w_gate, b_gate, ml_norm, ml_skip, w_out, norm2, w_router,
              w_e_gate, w_e_up, w_e_down, final_norm):
    angles = axial_angles(x.shape[1])
    for l in range(DEPTH):
        last = l == DEPTH - 1
        mod_l = jax.nn.silu(c) @ w_mod[l] + b_mod[l]
        mod_c = jax.nn.silu(c_ctx)[None, :] @ w_mod[l] + b_mod[l]
        sh1, sc1, g1, sh2, sc2, g2 = jnp.split(mod_l, 6, axis=-1)
        sh1c, sc1c, g1c, sh2c, sc2c, g2c = jnp.split(mod_c, 6, axis=-1)
        h_l = modulate(rmsnorm(x, norm1[l]), sh1, sc1)
        h_c = modulate(rmsnorm(ctx, norm1[l]), sh1c, sc1c)
        out_c, out_l = mixer(h_c, h_l, angles, last, w_in[l], q_norm[l], w_uq[l], kv_norm[l], w_ukv[l],
                             conv_w[l], conv_b[l], w_qblk[l], w_kblk[l], w_vblk[l], w_gate[l], b_gate[l],
                             ml_norm[l], ml_skip[l], w_out[l])
        x = x + g1[:, None, :] * out_l
        h_l = modulate(rmsnorm(x, norm2[l]), sh2, sc2)
        x = x + g2[:, None, :] * ec_moe(h_l, w_router[l], w_e_gate[l], w_e_up[l], w_e_down[l])
        if not last:
            ctx = ctx + g1c[:, None, :] * out_c
            h_c = modulate(rmsnorm(ctx, norm2[l]), sh2c, sc2c)
            ctx = ctx + g2c[:, None, :] * ec_moe(h_c, w_router[l], w_e_gate[l], w_e_up[l], w_e_down[l])
    return rmsnorm(x, final_norm)
```

```python
import os
import numpy as np
from contextlib import ExitStack, contextmanager
import concourse.bass as bass
import concourse.mybir as mybir
from concourse.bass_utils import run_bass_kernel_spmd

F32 = mybir.dt.float32
BF16 = mybir.dt.bfloat16
I32 = mybir.dt.int32
ALU = mybir.AluOpType
AF = mybir.ActivationFunctionType
AX = mybir.AxisListType

D = 1024
T = 8192
TC = 256
TA = T + TC
NEXP = 16
CAP = 1024
EPS = 1e-6
SEM_EPOCH = 30000
NEG = -1.0e30


class Buf:
    __slots__ = ("name", "last_w", "readers", "lane", "excl")

    def __init__(self, name, excl=False):
        self.name = name
        self.excl = excl
        self.last_w = None
        self.readers = []
        self.lane = None


class Op:
    __slots__ = ("eng", "fn", "deps", "is_dma", "lane", "lane_seq", "sig", "sig_idx", "idx")


class Prog:
    ENGS = ("pe", "act", "dve", "pool", "sp")

    def __init__(self, nc, stack, debug=()):
        self.nc = nc
        self.stack = stack
        self.cur = stack
        self.ops = []
        self.lanes = []
        self.n_tiles = 0
        self.epoch = None
        self.since = []
        self.debug = set(debug)
        self.free_lanes = []
        self.scope_lanes = [[]]

    def sb(self, shape, dtype, name=None):
        self.n_tiles += 1
        name = (name or "t") + f"_{self.n_tiles}"
        t = self.cur.enter_context(self.nc.sbuf_tensor(name, list(shape), dtype))
        return t, Buf(name)

    def ps(self, shape, dtype, name=None):
        self.n_tiles += 1
        name = (name or "p") + f"_{self.n_tiles}"
        t = self.cur.enter_context(self.nc.psum_tensor(name, list(shape), dtype))
        return t, Buf(name, excl=True)

    def dram(self, name, shape, dtype):
        kind = "ExternalOutput" if name in self.debug else "Internal"
        t = self.nc.dram_tensor(name, list(shape), dtype, kind=kind)
        return t.ap(), Buf(name)

    @contextmanager
    def scope(self):
        prev = self.cur
        st = ExitStack()
        self.cur = st
        self.scope_lanes.append([])
        try:
            yield
        finally:
            self.barrier()
            st.close()
            self.cur = prev
            self.free_lanes.extend(self.scope_lanes.pop())

    def _deps(self, op, reads, writes):
        ex = [r for r in reads if r.excl]
        if ex:
            reads = [r for r in reads if not r.excl]
            writes = list(writes) + [r for r in ex if r not in writes]
        deps = set()
        if self.epoch is not None:
            deps.add(self.epoch)
        for r in reads:
            if r.last_w is not None:
                deps.add(r.last_w)
        for w in writes:
            if w.last_w is not None:
                deps.add(w.last_w)
            for rd in w.readers:
                deps.add(rd)
        deps.discard(op)
        for r in reads:
            r.readers.append(op)
        for w in writes:
            w.last_w = op
            w.readers = []
        op.deps = deps

    def op(self, eng, fn, reads=(), writes=()):
        o = Op()
        o.eng = eng
        o.fn = fn
        o.is_dma = False
        o.lane = None
        o.lane_seq = 0
        o.sig = False
        o.idx = len(self.ops)
        self._deps(o, reads, writes)
        self.ops.append(o)
        self.since.append(o)
        return o

    def dma(self, queue, out, in_, reads=(), writes=(), lane_buf=None, fn=None, **kw):
        o = Op()
        o.eng = queue
        o.is_dma = True
        if lane_buf.lane is None:
            lane_buf.lane = {}
        if queue not in lane_buf.lane:
            fl = [l for l in self.free_lanes if l[0] == queue]
            if fl:
                self.free_lanes.remove(fl[0])
                lane_buf.lane[queue] = fl[0][1]
            else:
                lane_buf.lane[queue] = len(self.lanes)
                self.lanes.append([None, 0])
            self.scope_lanes[-1].append((queue, lane_buf.lane[queue]))
        o.lane = lane_buf.lane[queue]
        self.lanes[o.lane][1] += 1
        o.lane_seq = self.lanes[o.lane][1]
        o.sig = True
        o.idx = len(self.ops)
        o.fn = fn if fn is not None else (lambda e: e.dma_start(out=out, in_=in_, **kw))
        self._deps(o, reads, writes)
        self.ops.append(o)
        self.since.append(o)
        return o

    def barrier(self):
        if not hasattr(self, "_bar"):
            self._bar, self._bar_b = self.sb_root([128, 8], F32, "bar")
        prev = list(self.since)
        a = self._bar[:]
        o = self.op("pool", lambda e: e.memset(a, 0.0), writes=[self._bar_b])
        o.deps |= set(prev)
        o.deps.discard(o)
        self.epoch = o
        self.since = []
        return o

    def sb_root(self, shape, dtype, name):
        self.n_tiles += 1
        t = self.stack.enter_context(self.nc.sbuf_tensor(f"{name}_{self.n_tiles}", list(shape), dtype))
        return t, Buf(name)

    def mm(self, out, lhsT, rhs, start, stop, reads, writes):
        return self.op("pe", lambda e: e.matmul(out, lhsT=lhsT, rhs=rhs, start=start, stop=stop), reads, writes)

    def tr(self, out, in_, ident, reads, writes):
        return self.op("pe", lambda e: e.transpose(out=out, in_=in_, identity=ident), reads, writes)

    def act(self, out, in_, func, reads, writes, **kw):
        return self.op("act", lambda e: e.activation(out=out, in_=in_, func=func, **kw), reads, writes)

    def tt(self, eng, out, in0, in1, op, reads, writes):
        return self.op(eng, lambda e: e.tensor_tensor(out=out, in0=in0, in1=in1, op=op), reads, writes)

    def ts(self, eng, out, in0, s1, s2, op0, op1, reads, writes, **kw):
        if op1 is None:
            return self.op(eng, lambda e: e.tensor_scalar(out=out, in0=in0, scalar1=s1, scalar2=None, op0=op0, **kw), reads, writes)
        return self.op(eng, lambda e: e.tensor_scalar(out=out, in0=in0, scalar1=s1, scalar2=s2, op0=op0, op1=op1, **kw), reads, writes)

    def stt(self, eng, out, in0, scalar, in1, op0, op1, reads, writes):
        return self.op(eng, lambda e: e.scalar_tensor_tensor(out=out, in0=in0, scalar=scalar, in1=in1, op0=op0, op1=op1), reads, writes)

    def cp(self, eng, out, in_, reads, writes):
        if eng == "act":
            return self.op("act", lambda e: e.copy(out=out, in_=in_), reads, writes)
        return self.op(eng, lambda e: e.tensor_copy(out=out, in_=in_), reads, writes)

    def memset(self, eng, out, val, writes):
        return self.op(eng, lambda e: e.memset(out, val), (), writes)

    def emit(self, final_wait_ops=()):
        nc = self.nc
        ops = self.ops
        for o in ops:
            for d in o.deps:
                if not d.is_dma:
                    if d.eng == "pe" and o.eng == "pe" and not o.is_dma:
                        continue
                    d.sig = True
        for d in final_wait_ops:
            d.sig = True
        counts = {e: 0 for e in self.ENGS}
        for o in ops:
            if not o.is_dma and o.sig:
                counts[o.eng] += 1
                o.sig_idx = counts[o.eng]
        esems = {}
        for e in self.ENGS:
            n_ep = counts[e] // SEM_EPOCH + 1
            esems[e] = [self.stack.enter_context(nc.semaphore(f"s_{e}{k}")) for k in range(n_ep)]
        for k, l in enumerate(self.lanes):
            l[0] = self.stack.enter_context(nc.semaphore(f"l{k}"))
        self.n_sems = sum(len(v) for v in esems.values()) + len(self.lanes)

        def target(d):
            if d.is_dma:
                return (("l", d.lane), self.lanes[d.lane][0], 16 * d.lane_seq)
            ep, v = divmod(d.sig_idx - 1, SEM_EPOCH)
            return ((d.eng, ep), esems[d.eng][ep], v + 1)

        per_eng = {e: [o for o in ops if o.eng == e] for e in self.ENGS}
        block = self.stack.enter_context(nc.Block())

        def run(engname, e):
            waited = {}
            if engname == "pool":
                self.pool_reg = e.to_reg(NEXP * CAP - 1)
            for o in per_eng[engname]:
                need = {}
                for d in o.deps:
                    if (not d.is_dma) and (not o.is_dma) and d.eng == "pe" and o.eng == "pe":
                        continue
                    key, s, v = target(d)
                    if v > need.get(key, (None, 0))[1]:
                        need[key] = (s, v)
                for key, (s, v) in need.items():
                    if waited.get(key, 0) >= v:
                        continue
                    e.wait_ge(s, v)
                    waited[key] = v
                ins = o.fn(e)
                if o.is_dma:
                    ins.then_inc(self.lanes[o.lane][0], 16)
                elif o.sig:
                    ep = (o.sig_idx - 1) // SEM_EPOCH
                    ins.then_inc(esems[o.eng][ep], 1)
            if engname == "sp":
                for d in final_wait_ops:
                    key, s, v = target(d)
                    e.wait_ge(s, v)

        @block.tensor
        def _(e):
            run("pe", e)

        @block.scalar
        def _(e):
            run("act", e)

        @block.vector
        def _(e):
            run("dve", e)

        @block.gpsimd
        def _(e):
            run("pool", e)

        @block.sync
        def _(e):
            run("sp", e)


class RR:
    def __init__(self, items):
        self.items = items
        self.i = 0

    def next(self):
        it = self.items[self.i % len(self.items)]
        self.i += 1
        return it


def build(debug=(), stages=99):
    nc = bass.Bass("TRN2", target_bir_lowering=False)
    stack = ExitStack()
    P = Prog(nc, stack, debug)
    inp = {}

    def ext_in(name, shape, dtype=F32):
        inp[name] = nc.dram_tensor(name, list(shape), dtype, kind="ExternalInput").ap()
        return inp[name]

    xcat = ext_in("xcat", [TA, D])
    cvec = ext_in("cvec", [128, 8, 2])
    w_mod = ext_in("w_mod", [D, 6 * D])
    rowv = ext_in("rowv", [2, 8 * D])
    colv = ext_in("colv", [128, 8, 9])
    qkn = ext_in("qkn", [128, 5])
    w_in = ext_in("w_in", [D, 4864])
    w_uq = ext_in("w_uq", [384, 2048])
    w_ukv = ext_in("w_ukv", [256, 2048])
    ropet = ext_in("ropet", [2, 128, TA])
    cst = ext_in("cst", [128, 8, 128])
    w_bd = ext_in("w_bd", [128, 24, 128])
    w_gate = ext_in("w_gate", [128, 24, 16])
    b_gate = ext_in("b_gate", [16, 1])
    w_out = ext_in("w_out", [D, D])
    w_router = ext_in("w_router", [128, 8, 16])
    w_eg = ext_in("w_eg", [NEXP, D, D])
    w_eu = ext_in("w_eu", [NEXP, D, D])
    w_ed = ext_in("w_ed", [NEXP, D, D])
    tokid = ext_in("tokid", [128, 64], I32)
    out = nc.dram_tensor("out", [T, D], F32, kind="ExternalOutput").ap()

    XOFF_C, XOFF_L = 2, 262
    XMT, XMT_b = P.dram("XMT", [D, 8456], F32)
    ZS, ZS_b = P.dram("ZS", [D, T], BF16)
    GA, GA_b = P.dram("GA", [D, T], BF16)
    GM, GM_b = P.dram("GM", [D, T], BF16)
    QN, QN_b = P.dram("QN", [D, T], BF16)
    QR, QR_b = P.dram("QR", [512, T], BF16)
    KN, KN_b = P.dram("KN", [D, TA], BF16)
    KR, KR_b = P.dram("KR", [64, TA], BF16)
    VV, VV_b = P.dram("VV", [TA, D], BF16)
    XCT, XCT_b = P.dram("XCT", [D, T], F32)
    MQT, MQT_b = P.dram("MQT", [D, T], BF16)
    MKT, MKT_b = P.dram("MKT", [D, TA], BF16)
    MK, MK_b = P.dram("MK", [TA, D], BF16)
    MV, MV_b = P.dram("MV", [TA, D], BF16)
    OT, OT_b = P.dram("OT", [D, T], F32)
    HF, HF_b = P.dram("HF", [T, D], F32)
    HNT, HNT_b = P.dram("HNT", [D, T], F32)
    X1, X1_b = P.dram("X1", [T, D], F32)
    H2, H2_b = P.dram("H2", [T, D], BF16)
    XS, XS_b = P.dram("XS", [NEXP * CAP, D], BF16)
    YY, YY_b = P.dram("YY", [NEXP * CAP, D], F32)

    finals = []
    with stack:
        P._bar, P._bar_b = P.sb_root([128, 8], F32, "bar")
        cs, cs_b = P.sb([128, 8, 128], F32, "cst")
        csb, csb_b = P.sb([128, 8, 128], BF16, "cstb")
        colt, colt_b = P.sb([128, 8, 9], F32, "colv")
        qknt, qknt_b = P.sb([128, 5], F32, "qkn")
        modc, modc_b = P.sb([128, 48, 2], F32, "modc")
        s1c, s1c_b = P.sb([128, 8, 2], F32, "s1c")
        bc, bc_b = P.sb([128, 5, D], F32, "bc")
        gtm, gtm_b = P.sb([128, 66, 16], F32, "gtm")
        aff, aff_b = P.sb([128, 64, 16], F32, "aff")
        posi, posi_b = P.sb([128, 64 * 16], I32, "posi")
        gmv, gmv_b = P.sb([128, 64, 16], F32, "gmv")
        psb = [P.ps([128, 512], F32, f"ps{i}") for i in range(7)]
        psbf, psbf_b = P.ps([128, 1024], BF16, "psbf")
        P.dma("sp", cs[:], cst, writes=[cs_b], lane_buf=cs_b)
        P.dma("sp", colt[:], colv, writes=[colt_b], lane_buf=colt_b)
        P.dma("sp", qknt[:], qkn, writes=[qknt_b], lane_buf=qknt_b)
        P.cp("dve", csb[:], cs[:], [cs_b], [csb_b])
        ident = cs[:, 0, :]
        onesb = csb[:, 7, :]
        psr = RR(psb)

        with P.scope():
            cv, cv_b = P.sb([128, 8, 2], F32, "cv")
            sv, sv_b = P.sb([128, 8, 2], F32, "sv")
            rv, rv_b = P.sb([2, 8 * D], F32, "rv")
            mrow, mrow_b = P.sb([2, 8 * D], F32, "mrow")
            wms = [P.sb([128, 8, 512], F32, f"wm{i}") for i in range(2)]
            P.dma("sp", cv[:], cvec, writes=[cv_b], lane_buf=cv_b)
            P.dma("sp", rv[:], rowv, writes=[rv_b], lane_buf=rv_b)
            P.act(sv[:], cv[:], AF.Silu, [cv_b], [sv_b])
            for j in range(12):
                wm, wm_b = wms[j % 2]
                P.dma("sp" if j % 2 == 0 else "pool", wm[:], w_mod[:, j * 512:(j + 1) * 512].rearrange("(c p) n -> p c n", p=128),
                      writes=[wm_b], lane_buf=wm_b)
                pt, pt_b = psr.next()
                for k in range(8):
                    P.mm(pt[0:2, :], sv[:, k, :], wm[:, k, :], k == 0, k == 7, [sv_b, wm_b], [pt_b])
                P.tt("dve", mrow[:, j * 512:(j + 1) * 512], pt[0:2, :], rv[:, j * 512:(j + 1) * 512], ALU.add, [pt_b, rv_b], [mrow_b])
            P.cp("dve", mrow[:, 6 * D:8 * D], rv[:, 6 * D:8 * D], [rv_b], [mrow_b])
            for g in range(6):
                pt, pt_b = psr.next()
                for c in range(8):
                    j = g * 8 + c
                    P.tr(pt[:, c * 2:c * 2 + 2], mrow[:, j * 128:(j + 1) * 128], cs[0:2, 0, 0:2], [mrow_b, cs_b], [pt_b])
                P.cp("dve", modc[:, g * 8:(g + 1) * 8, :].rearrange("p c v -> p (c v)"), pt[:, 0:16], [pt_b], [modc_b])
            for v in range(2):
                P.stt("dve", s1c[:, :, v], modc[:, 8:16, v], 1.0, colt[:, :, 0], ALU.add, ALU.mult, [modc_b, colt_b], [s1c_b])
            srcs = [2 * D, 4 * D, 3 * D, 5 * D, 7 * D, 6 * D]
            n2b, n2b_b = P.sb([128, D], F32, "n2b")
            for i, off in enumerate(srcs):
                for hh in range(2):
                    pt, pt_b = psr.next()
                    P.mm(pt[:, :], cs[0:2, 6, :], mrow[:, off + hh * 512: off + (hh + 1) * 512], True, True, [cs_b, mrow_b], [pt_b])
                    if i < 5:
                        P.cp("dve", bc[:, i, hh * 512:(hh + 1) * 512], pt[:, :], [pt_b], [bc_b])
                    else:
                        P.cp("dve", n2b[:, hh * 512:(hh + 1) * 512], pt[:, :], [pt_b], [n2b_b])
            P.stt("dve", bc[:, 1, :], bc[:, 1, :], 1.0, n2b[:], ALU.add, ALU.mult, [bc_b, n2b_b], [bc_b])
            if "DBG_mod" in P.debug:
                dm, dm_b = P.dram("DBG_mod", [128, 96], F32)
                finals.append(P.dma("sp", dm, modc[:].rearrange("p c v -> p (c v)"), reads=[modc_b], lane_buf=modc_b))
                db, db_b = P.dram("DBG_bc", [128, 5 * D], F32)
                finals.append(P.dma("sp", db, bc[:].rearrange("p c v -> p (c v)"), reads=[bc_b], lane_buf=bc_b))

        def stage1(pass_b):
          with P.scope():
            ncol = 3072 if pass_b else 1792
            win, win_b = P.sb([128, 8, ncol], BF16, "win")
            if not pass_b:
                wuq, wuq_b = P.sb([128, 3, 2048], BF16, "wuq")
                wukv, wukv_b = P.sb([128, 2, 2048], BF16, "wukv")
            xin = RR([P.sb([128, 4, D], F32, f"xin{i}") for i in range(2)])
            hT = RR([P.sb([128, 8, 512], BF16, f"hT{i}") for i in range(2)])
            sqj, sqj_b = P.sb([128, D], BF16, "sqj")
            ssr = RR([P.sb([128, 12], F32, f"ss{i}") for i in range(2)])
            dgr = RR([P.sb([128, 4, 128], F32, f"dg{i}") for i in range(2)])
            if not pass_b:
              qlf, qlf_b = P.sb([128, 5, 512], F32, "qlf")
              sqb, sqb_b = P.sb([128, 5, 512], BF16, "sqb")
              rq, rq_b = P.sb([128, 2, 512], F32, "rq")
              qn, qn_b = P.sb([128, 5, 512], BF16, "qn")
              rope, rope_b = P.sb([128, 2, 512], F32, "rope")
              rt1, rt1_b = P.sb([128, 512], F32, "rt1")
              rt2, rt2_b = P.sb([128, 512], F32, "rt2")
              ob16 = RR([P.sb([128, 512], BF16, f"ob{i}") for i in range(4)])
              vb16 = RR([P.sb([128, D], BF16, f"vb{i}") for i in range(2)])
              zpad, zpad_b = P.sb([128, 8, 2], F32, "zpad")
            of32 = RR([P.sb([128, 512], F32, f"of{i}") for i in range(4)])
            of16 = RR([P.sb([128, 512], BF16, f"of16{i}") for i in range(4)])
            dq = RR(["sp", "pool"] if "USEPOOL" in P.debug else ["sp"])

            srccols = [1728 + j * 256 for j in range(12)] if pass_b else ([j * 256 for j in range(6)] + [1536, 4800])
            dstcol = 0
            for j, sc_ in enumerate(srccols):
                w = 256
                if not pass_b and j == 6:
                    w = 192
                if not pass_b and j == 7:
                    w = 64
                P.dma("pool", win[:, :, dstcol:dstcol + w], w_in[:, sc_:sc_ + w].rearrange("(c p) n -> p c n", p=128), writes=[win_b], lane_buf=win_b)
                dstcol += w
            if not pass_b:
                P.memset("dve", zpad[:], 0.0, [zpad_b])
                for off in (0, 258, 260, 260 + 2 + T):
                    P.dma("sp", XMT[:, off:off + 2].rearrange("(c p) n -> p c n", p=128), zpad[:], reads=[zpad_b], writes=[XMT_b], lane_buf=zpad_b)
                for j in range(4):
                    P.dma("pool", wuq[:, :, j * 512:(j + 1) * 512], w_uq[:, j * 512:(j + 1) * 512].rearrange("(c p) n -> p c n", p=128), writes=[wuq_b], lane_buf=wuq_b)
                for j in range(4):
                    P.dma("pool", wukv[:, :, j * 512:(j + 1) * 512], w_ukv[:, j * 512:(j + 1) * 512].rearrange("(c p) n -> p c n", p=128), writes=[wukv_b], lane_buf=wukv_b)

            blocks = [(0, 256, True)] + [(TC + i * 512, 512, False) for i in range(T // 512)]
            LIM = int(os.environ.get("S1LIM", "99"))
            blocks = blocks[:int(os.environ.get("S1NBLK", "99"))]
            if LIM < 1:
                blocks = []
            def block_gen(t0, NT, is_ctx):
                nt = NT // 128
                var = 1 if is_ctx else 0
                tq = t0 - TC
                xoff = (XOFF_C + t0) if is_ctx else (XOFF_L + tq)
                xi, xi_b = xin.next()
                P.dma(dq.next(), xi[:, 0:nt, :], xcat[t0:t0 + NT, :].rearrange("(i p) f -> p i f", p=128), writes=[xi_b], lane_buf=xi_b)
                ss, ss_b = ssr.next()
                dg, dg_b = dgr.next()
                P.memset("dve", ss[:], 0.0, [ss_b])
                for i in range(nt):
                    P.act(sqj[:], xi[:, i, :], AF.Square, [xi_b, ss_b], [sqj_b, ss_b], accum_out=ss[:, i:i + 1])
                P.act(ss[:, 4:4 + nt], ss[:, 0:nt], AF.Ln, [ss_b], [ss_b], scale=1.0 / D, bias=EPS)
                P.act(ss[:, 8:8 + nt], ss[:, 4:4 + nt], AF.Exp, [ss_b], [ss_b], scale=-0.5)
                for i in range(nt):
                    P.ts("dve", dg[:, i, :], ident, ss[:, 8 + i:9 + i], None, ALU.mult, None, [cs_b, ss_b], [dg_b])
                h, h_b = hT.next()
                for c in range(8):
                    pt, pt_b = psr.next()
                    for i in range(nt):
                        P.mm(pt[:, i * 128:(i + 1) * 128], xi[:, i, c * 128:(c + 1) * 128], dg[:, i, :], True, True, [xi_b, dg_b], [pt_b])
                    P.act(h[:, c, 0:NT], pt[:, 0:NT], AF.Identity, [pt_b, s1c_b, modc_b], [h_b],
                          scale=s1c[:, c, var:var + 1], bias=modc[:, c, var:var + 1])

                yield
                if not pass_b:
                    P.dma(dq.next(), rope[:, :, 0:NT], ropet[:, :, t0:t0 + NT].rearrange("a p t -> p a t"), writes=[rope_b], lane_buf=rope_b)
                def gemm(col0, ncols, pt, pt_b):
                    for k in range(8):
                        P.mm(pt[0:ncols, 0:NT], win[:, k, col0:col0 + ncols], h[:, k, 0:NT], k == 0, k == 7, [win_b, h_b], [pt_b])

                if pass_b:
                    for gi, (dst, dst_b) in enumerate(((ZS, ZS_b), (GA, GA_b), (GM, GM_b))):
                        for c in range(8):
                            pt, pt_b = psr.next()
                            gemm(gi * 1024 + c * 128, 128, pt, pt_b)
                            of, of_b = of16.next()
                            P.act(of[:, 0:NT], pt[:, 0:NT], AF.Sigmoid, [pt_b], [of_b])
                            P.dma(dq.next(), dst[c * 128:(c + 1) * 128, tq:tq + NT], of[:, 0:NT], reads=[of_b], writes=[dst_b], lane_buf=of_b)
                    return
                if LIM < 2:
                    return
                lat = range(0, 5) if not is_ctx else range(3, 5)
                for c in lat:
                    pt, pt_b = psr.next()
                    gemm(c * 128, 128, pt, pt_b)
                    P.cp("dve", qlf[:, c, 0:NT], pt[:, 0:NT], [pt_b], [qlf_b])
                    P.act(sqb[:, c, 0:NT], pt[:, 0:NT], AF.Square, [pt_b], [sqb_b])
                groups = ([(0, 0, 3, 384.0)] if not is_ctx else []) + [(1, 3, 2, 256.0)]
                for (gi, c0, ncx, nfeat) in groups:
                    pt, pt_b = psr.next()
                    for k in range(ncx):
                        P.mm(pt[:, 0:NT], onesb, sqb[:, c0 + k, 0:NT], k == 0, k == ncx - 1, [csb_b, sqb_b], [pt_b])
                    P.act(rq[:, gi, 0:NT], pt[:, 0:NT], AF.Ln, [pt_b], [rq_b], scale=1.0 / nfeat, bias=EPS)
                    P.act(rq[:, gi, 0:NT], rq[:, gi, 0:NT], AF.Exp, [rq_b], [rq_b], scale=-0.5)
                    for k in range(ncx):
                        c = c0 + k
                        P.stt("dve", qn[:, c, 0:NT], qlf[:, c, 0:NT], qknt[:, c:c + 1], rq[:, gi, 0:NT], ALU.mult, ALU.mult,
                              [qlf_b, qknt_b, rq_b], [qn_b])
                if LIM < 3:
                    return
                if not is_ctx:
                    for hd in range(8):
                        pt, pt_b = psr.next()
                        for k in range(3):
                            P.mm(pt[:, 0:NT], wuq[:, k, hd * 128:(hd + 1) * 128], qn[:, k, 0:NT], k == 0, k == 2, [wuq_b, qn_b], [pt_b])
                        ob, ob_b = ob16.next()
                        P.cp("act", ob[:, 0:NT], pt[:, 0:NT], [pt_b], [ob_b])
                        P.dma(dq.next(), QN[hd * 128:(hd + 1) * 128, tq:tq + NT], ob[:, 0:NT], reads=[ob_b], writes=[QN_b], lane_buf=ob_b)
                    for hp in range(4):
                        p1, p1_b = psr.next()
                        p2, p2_b = psr.next()
                        for k in range(3):
                            P.mm(p1[:, 0:NT], wuq[:, k, 1024 + hp * 128:1024 + (hp + 1) * 128], qn[:, k, 0:NT], k == 0, k == 2, [wuq_b, qn_b], [p1_b])
                        for k in range(3):
                            P.mm(p2[:, 0:NT], wuq[:, k, 1536 + hp * 128:1536 + (hp + 1) * 128], qn[:, k, 0:NT], k == 0, k == 2, [wuq_b, qn_b], [p2_b])
                        P.tt("dve", rt1[:, 0:NT], p1[:, 0:NT], rope[:, 0, 0:NT], ALU.mult, [p1_b, rope_b], [rt1_b])
                        P.tt("dve", rt2[:, 0:NT], p2[:, 0:NT], rope[:, 1, 0:NT], ALU.mult, [p2_b, rope_b], [rt2_b])
                        ob, ob_b = ob16.next()
                        P.tt("pool", ob[:, 0:NT], rt1[:, 0:NT], rt2[:, 0:NT], ALU.add, [rt1_b, rt2_b], [ob_b])
                        P.dma(dq.next(), QR[hp * 128:(hp + 1) * 128, tq:tq + NT], ob[:, 0:NT], reads=[ob_b], writes=[QR_b], lane_buf=ob_b)
                if LIM < 4:
                    return
                for hd in range(8):
                    pt, pt_b = psr.next()
                    for k in range(2):
                        P.mm(pt[:, 0:NT], wukv[:, k, hd * 128:(hd + 1) * 128], qn[:, 3 + k, 0:NT], k == 0, k == 1, [wukv_b, qn_b], [pt_b])
                    ob, ob_b = ob16.next()
                    P.cp("act", ob[:, 0:NT], pt[:, 0:NT], [pt_b], [ob_b])
                    P.dma(dq.next(), KN[hd * 128:(hd + 1) * 128, t0:t0 + NT], ob[:, 0:NT], reads=[ob_b], writes=[KN_b], lane_buf=ob_b)
                if LIM < 5:
                    return
                for i in range(nt):
                    vb, vb_b = vb16.next()
                    for hh in range(2):
                        pt, pt_b = psr.next()
                        for k in range(2):
                            P.mm(pt[:, :], qn[:, 3 + k, i * 128:(i + 1) * 128], wukv[:, k, 1024 + hh * 512:1024 + (hh + 1) * 512], k == 0, k == 1,
                                 [qn_b, wukv_b], [pt_b])
                        P.cp("act" if hh else "dve", vb[:, hh * 512:(hh + 1) * 512], pt[:, :], [pt_b], [vb_b])
                    P.dma(dq.next(), VV[t0 + i * 128:t0 + (i + 1) * 128, :], vb[:], reads=[vb_b], writes=[VV_b], lane_buf=vb_b)
                if LIM < 6:
                    return
                p1, p1_b = psr.next()
                p2, p2_b = psr.next()
                gemm(640, 64, p1, p1_b)
                gemm(1728, 64, p2, p2_b)
                P.tt("dve", rt1[0:64, 0:NT], p1[0:64, 0:NT], rope[0:64, 0, 0:NT], ALU.mult, [p1_b, rope_b], [rt1_b])
                P.tt("dve", rt2[0:64, 0:NT], p2[0:64, 0:NT], rope[0:64, 1, 0:NT], ALU.mult, [p2_b, rope_b], [rt2_b])
                ob, ob_b = ob16.next()
                P.tt("pool", ob[0:64, 0:NT], rt1[0:64, 0:NT], rt2[0:64, 0:NT], ALU.add, [rt1_b, rt2_b], [ob_b])
                P.dma(dq.next(), KR[:, t0:t0 + NT], ob[0:64, 0:NT], reads=[ob_b], writes=[KR_b], lane_buf=ob_b)
                if LIM < 7:
                    return
                for c in range(8):
                    pt, pt_b = psr.next()
                    gemm(704 + c * 128, 128, pt, pt_b)
                    of, of_b = of32.next()
                    P.cp("act" if c % 2 else "dve", of[:, 0:NT], pt[:, 0:NT], [pt_b], [of_b])
                    P.dma(dq.next(), XMT[c * 128:(c + 1) * 128, xoff:xoff + NT], of[:, 0:NT], reads=[of_b], writes=[XMT_b], lane_buf=of_b)

            todo = [b for b in blocks if not (pass_b and b[2])]
            gens = [block_gen(*b) for b in todo]
            if gens:
                next(gens[0])
            for gi_ in range(len(gens)):
                if gi_ + 1 < len(gens):
                    next(gens[gi_ + 1])
                for _ in gens[gi_]:
                    pass

        ST = set(os.environ.get("STAGES", "1a,1b,2,attn,scan,merge,moe,final").split(","))
        if "1a" in ST:
            stage1(False)
        if "1b" in ST:
            stage1(True)


        def stage2():
          with P.scope():
            wst, wst_b = P.sb([128, 24, 128], F32, "wst")
            wbd16, wbd16_b = P.sb([128, 24, 128], BF16, "wbd16")
            wgs, wgs_b = P.sb([128, 24, 16], F32, "wgs")
            wg16, wg16_b = P.sb([128, 24, 16], BF16, "wg16")
            bg, bg_b = P.sb([16, 1], F32, "bg")
            xm32 = RR([P.sb([128, 516], F32, f"xm32{i}") for i in range(4)])
            accr = RR([P.sb([128, 512], F32, f"acc{i}") for i in range(2)])
            xcfr = RR([P.sb([128, 512], F32, f"xcf{i}") for i in range(2)])
            xcbr = RR([P.sb([128, 512], BF16, f"xcb{i}") for i in range(2)])
            xmbr = RR([P.sb([128, 512], BF16, f"xmb{i}") for i in range(2)])
            qtbr = RR([P.sb([128, 512], BF16, f"qtb{i}") for i in range(2)])
            ktsr = RR([P.sb([128, 512], BF16, f"kts{i}") for i in range(2)])
            vtbr = RR([P.sb([128, 512], BF16, f"vtb{i}") for i in range(2)])
            ktmr = RR([P.sb([128, 512], BF16, f"ktm{i}") for i in range(2)])
            vtmr = RR([P.sb([128, 512], BF16, f"vtm{i}") for i in range(2)])
            gts, gts_b = P.sb([16, 512], F32, "gts")
            lft, lft_b = P.sb([128, 66, 4], F32, "lft")
            dq = RR(["sp"])
            psr2 = RR(psb[0:6])
            pg, pg_b = psb[6]
            P.memset("dve", gtm[:], 0.0, [gtm_b])
            P.dma("sp", wst[:], w_bd, writes=[wst_b], lane_buf=wst_b)
            P.cp("dve", wbd16[:], wst[:], [wst_b], [wbd16_b])
            P.dma("sp", wgs[:], w_gate, writes=[wgs_b], lane_buf=wgs_b)
            P.dma("sp", bg[:], b_gate, writes=[bg_b], lane_buf=bg_b)
            P.cp("dve", wg16[:, 0:8, :], wgs[:, 0:8, :], [wgs_b], [wg16_b])
            P.ts("dve", wg16[:, 8:16, :], wgs[:, 8:16, :], 16.0, None, ALU.mult, None, [wgs_b], [wg16_b])
            P.cp("dve", wg16[:, 16:24, :], wgs[:, 16:24, :], [wgs_b], [wg16_b])
            blocks = [(0, 256, True)] + [(TC + i * 512, 512, False) for i in range(T // 512)]
            blocks = blocks[:int(os.environ.get("S2NBLK", "99"))]

            def chunk_gen(t0, NT, is_ctx, c):
                nt = NT // 128
                tq = t0 - TC
                xoff = (XOFF_C + t0) if is_ctx else (XOFF_L + tq)
                xm, xm_b = xm32.next()
                P.dma(dq.next(), xm[:, 0:NT + 4], XMT[c * 128:(c + 1) * 128, xoff - 2:xoff + NT + 2], reads=[XMT_b], writes=[xm_b], lane_buf=xm_b)
                acc, acc_b = accr.next()
                P.ts("dve", acc[:, 0:NT], xm[:, 0:NT], colt[:, c, 4:5], colt[:, c, 1:2], ALU.mult, ALU.add, [xm_b, colt_b], [acc_b])
                for j in range(1, 5):
                    P.stt("dve", acc[:, 0:NT], xm[:, j:j + NT], colt[:, c, 4 + j:5 + j], acc[:, 0:NT], ALU.mult, ALU.add, [xm_b, colt_b, acc_b], [acc_b])
                xcf, xcf_b = xcfr.next()
                P.act(xcf[:, 0:NT], acc[:, 0:NT], AF.Silu, [acc_b], [xcf_b])
                if not is_ctx:
                    P.dma(dq.next(), XCT[c * 128:(c + 1) * 128, tq:tq + NT], xcf[:, 0:NT], reads=[xcf_b], writes=[XCT_b], lane_buf=xcf_b)
                xcb, xcb_b = xcbr.next()
                xmb, xmb_b = xmbr.next()
                P.cp("act", xcb[:, 0:NT], xcf[:, 0:NT], [xcf_b], [xcb_b])
                P.cp("act", xmb[:, 0:NT], xm[:, 2:2 + NT], [xm_b], [xmb_b])
                yield
                qtb, qtb_b = qtbr.next()
                kts, kts_b = ktsr.next()
                vtb, vtb_b = vtbr.next()
                pt, pt_b = psr2.next()
                P.mm(pt[:, 0:NT], wbd16[:, c, :], xcb[:, 0:NT], True, True, [wbd16_b, xcb_b], [pt_b])
                P.cp("act", qtb[:, 0:NT], pt[:, 0:NT], [pt_b], [qtb_b])
                if not is_ctx:
                    P.dma(dq.next(), MQT[c * 128:(c + 1) * 128, tq:tq + NT], qtb[:, 0:NT], reads=[qtb_b], writes=[MQT_b], lane_buf=qtb_b)
                pt, pt_b = psr2.next()
                P.mm(pt[:, 0:NT], wbd16[:, 8 + c, :], xcb[:, 0:NT], True, True, [wbd16_b, xcb_b], [pt_b])
                P.act(kts[:, 0:NT], pt[:, 0:NT], AF.Copy, [pt_b], [kts_b], scale=1.0 / 16.0)
                P.dma(dq.next(), MKT[c * 128:(c + 1) * 128, t0:t0 + NT], kts[:, 0:NT], reads=[kts_b], writes=[MKT_b], lane_buf=kts_b)
                pt, pt_b = psr2.next()
                P.mm(pt[:, 0:NT], wbd16[:, 16 + c, :], xmb[:, 0:NT], True, True, [wbd16_b, xmb_b], [pt_b])
                P.cp("dve", vtb[:, 0:NT], pt[:, 0:NT], [pt_b], [vtb_b])
                P.mm(pg[0:16, 0:NT], wg16[:, c, :], qtb[:, 0:NT], c == 0, False, [wg16_b, qtb_b], [pg_b])
                P.mm(pg[0:16, 0:NT], wg16[:, 8 + c, :], kts[:, 0:NT], False, False, [wg16_b, kts_b], [pg_b])
                P.mm(pg[0:16, 0:NT], wg16[:, 16 + c, :], vtb[:, 0:NT], False, c == 7, [wg16_b, vtb_b], [pg_b])
                ktm, ktm_b = ktmr.next()
                vtm, vtm_b = vtmr.next()
                pt, pt_b = psr2.next()
                for i in range(nt):
                    P.mm(pt[:, i * 128:(i + 1) * 128], xcb[:, i * 128:(i + 1) * 128], wbd16[:, 8 + c, :], True, True, [xcb_b, wbd16_b], [pt_b])
                P.act(ktm[:, 0:NT], pt[:, 0:NT], AF.Copy, [pt_b], [ktm_b], scale=1.0 / 16.0)
                P.dma(dq.next(), MK[t0:t0 + NT, c * 128:(c + 1) * 128].rearrange("(i p) d -> p i d", p=128),
                      ktm[:, 0:NT].rearrange("p (i d) -> p i d", d=128), reads=[ktm_b], writes=[MK_b], lane_buf=ktm_b)
                pt, pt_b = psr2.next()
                for i in range(nt):
                    P.mm(pt[:, i * 128:(i + 1) * 128], xmb[:, i * 128:(i + 1) * 128], wbd16[:, 16 + c, :], True, True, [xmb_b, wbd16_b], [pt_b])
                P.cp("dve", vtm[:, 0:NT], pt[:, 0:NT], [pt_b], [vtm_b])
                P.dma(dq.next(), MV[t0:t0 + NT, c * 128:(c + 1) * 128].rearrange("(i p) d -> p i d", p=128),
                      vtm[:, 0:NT].rearrange("p (i d) -> p i d", d=128), reads=[vtm_b], writes=[MV_b], lane_buf=vtm_b)
                if c == 7:
                    P.act(gts[:, 0:NT], pg[0:16, 0:NT], AF.Identity, [pg_b, bg_b], [gts_b], bias=bg[:, 0:1])
                    pt, pt_b = psr2.next()
                    for i in range(nt):
                        P.tr(pt[:, i * 16:(i + 1) * 16], gts[:, i * 128:(i + 1) * 128], cs[0:16, 0, 0:16], [gts_b, cs_b], [pt_b])
                    j0 = t0 // 128
                    P.cp("dve", gtm[:, j0:j0 + nt, :], pt[:, 0:nt * 16].rearrange("p (i g) -> p i g", g=16), [pt_b], [gtm_b])

            gens = [chunk_gen(t0, NT, is_ctx, c) for (t0, NT, is_ctx) in blocks for c in range(8)]
            if gens:
                next(gens[0])
            for gi_ in range(len(gens)):
                if gi_ + 1 < len(gens):
                    next(gens[gi_ + 1])
                for _ in gens[gi_]:
                    pass
            for d_ in range(2):
                fv = gtm[:, :, d_ * 8 + 4:d_ * 8 + 8]
                P.act(lft[:], fv, AF.Exp, [gtm_b], [lft_b], scale=-1.0)
                P.act(lft[:], lft[:], AF.Ln, [lft_b], [lft_b], bias=1.0)
                P.ts("dve", fv, lft[:], -1.0, None, ALU.mult, None, [lft_b], [gtm_b])
            if "DBG_gtm" in P.debug:
                dg_, dg_b_ = P.dram("DBG_gtm", [128, 66 * 16], F32)
                finals.append(P.dma("sp", dg_, gtm[:].rearrange("p j g -> p (j g)"), reads=[gtm_b], lane_buf=gtm_b))

        def stage_attn():
          with P.scope():
            KA, KA_b = P.sb([128, TA], BF16, "KA")
            KB, KB_b = P.sb([128, TA], BF16, "KB")
            Vt, Vt_b = P.sb([128, 66, 128], BF16, "Vt")
            QA, QA_b = P.sb([128, T], BF16, "QA")
            QB, QB_b = P.sb([128, T], BF16, "QB")
            pTr = RR([P.sb([128, 512], BF16, f"pT{i}") for i in range(6)])
            s2r = RR([P.sb([128, 512], BF16, f"ps2{i}") for i in range(4)])
            s4r = RR([P.sb([128, 512], BF16, f"ps4{i}") for i in range(3)])
            rinvr = RR([P.sb([128, 512], F32, f"rinv{i}") for i in range(2)])
            osbr = RR([P.sb([128, 512], F32, f"osb{i}") for i in range(2)])
            Sr = RR(psb[0:4])
            Or = RR(psb[4:6])
            Mr = RR(psb[6:7])
            NH = int(os.environ.get("ATT_NH", "8"))
            NQG = int(os.environ.get("ATT_NQG", "16"))
            scale = float((128 + 64) ** -0.5)
            for c0 in range(0, TA, 2112):
                P.memset("dve", KB[64:128, c0:c0 + 2112], 0.0, [KB_b])
            for c0 in range(0, T, 2048):
                P.memset("dve", QB[64:128, c0:c0 + 2048], 0.0, [QB_b])
            P.dma("sp", KB[0:64, 0:4224], KR[:, 0:4224], reads=[KR_b], writes=[KB_b], lane_buf=KB_b)
            P.dma("sp", KB[0:64, 4224:TA], KR[:, 4224:TA], reads=[KR_b], writes=[KB_b], lane_buf=KB_b)
            for h in range(NH):
                P.dma("sp", KA[:, 0:4224], KN[h * 128:(h + 1) * 128, 0:4224], reads=[KN_b], writes=[KA_b], lane_buf=KA_b)
                P.dma("sp", KA[:, 4224:TA], KN[h * 128:(h + 1) * 128, 4224:TA], reads=[KN_b], writes=[KA_b], lane_buf=KA_b)
                P.dma("sp", QA[:, 0:4096], QN[h * 128:(h + 1) * 128, 0:4096], reads=[QN_b], writes=[QA_b], lane_buf=QA_b)
                P.dma("sp", QA[:, 4096:T], QN[h * 128:(h + 1) * 128, 4096:T], reads=[QN_b], writes=[QA_b], lane_buf=QA_b)
                P.dma("sp", QB[0:64, :], QR[h * 64:(h + 1) * 64, :], reads=[QR_b], writes=[QB_b], lane_buf=QB_b)
                P.dma("sp", Vt[:, 0:33, :], VV[0:4224, h * 128:(h + 1) * 128].rearrange("(j p) d -> p j d", p=128), reads=[VV_b], writes=[Vt_b], lane_buf=Vt_b)
                P.dma("sp", Vt[:, 33:66, :], VV[4224:TA, h * 128:(h + 1) * 128].rearrange("(j p) d -> p j d", p=128), reads=[VV_b], writes=[Vt_b], lane_buf=Vt_b)
                for qg in range(NQG):
                    q0 = qg * 512
                    O, O_b = Or.next()
                    M, M_b = Mr.next()

                    def qk(j):
                        S, S_b = Sr.next()
                        P.mm(S[:, :], KA[:, j * 128:(j + 1) * 128], QA[:, q0:q0 + 512], True, False, [KA_b, QA_b], [S_b])
                        P.mm(S[:, :], KB[:, j * 128:(j + 1) * 128], QB[:, q0:q0 + 512], False, True, [KB_b, QB_b], [S_b])
                        return S, S_b
                    sq_ = [qk(0), qk(1)]
                    prev_pT = None
                    hold = None
                    pend = []
                    nsum = 0
                    for j in range(66):
                        if j + 2 < 66:
                            sq_.append(qk(j + 2))
                        S, S_b = sq_.pop(0)
                        pT, pT_b = pTr.next()
                        P.act(pT[:, :], S[:, :], AF.Exp, [S_b], [pT_b], scale=scale)
                        P.mm(O[:, :], Vt[:, j, :], pT[:, :], j == 0, j == 65, [Vt_b, pT_b], [O_b])
                        if j % 2 == 1:
                            if pend and (j % 4 == 1):
                                s4, s4_b = pend.pop(0)
                                P.mm(M[:, :], onesb, s4[:, :], nsum == 0, False, [csb_b, s4_b], [M_b])
                                nsum += 1
                            s2, s2_b = s2r.next()
                            P.tt("dve", s2[:, :], prev_pT[0][:, :], pT[:, :], ALU.add, [prev_pT[1], pT_b], [s2_b])
                            if j % 4 == 3:
                                s4, s4_b = s4r.next()
                                P.tt("dve", s4[:, :], hold[0][:, :], s2[:, :], ALU.add, [hold[1], s2_b], [s4_b])
                                pend.append((s4, s4_b))
                            elif j == 65:
                                pend.append((s2, s2_b))
                            else:
                                hold = (s2, s2_b)
                        prev_pT = (pT, pT_b)
                    while pend:
                        s4, s4_b = pend.pop(0)
                        P.mm(M[:, :], onesb, s4[:, :], nsum == 0, len(pend) == 0, [csb_b, s4_b], [M_b])
                        nsum += 1
                    rinv, rinv_b = rinvr.next()
                    osb, osb_b = osbr.next()
                    P.op("dve", (lambda e, a=rinv[:, :], b=M[:, :]: e.reciprocal(out=a, in_=b)), [M_b], [rinv_b])
                    P.tt("dve", osb[:, :], O[:, :], rinv[:, :], ALU.mult, [O_b, rinv_b], [osb_b])
                    P.dma("sp", OT[h * 128:(h + 1) * 128, q0:q0 + 512], osb[:, :], reads=[osb_b], writes=[OT_b], lane_buf=osb_b)


        def stage_scan():
          with P.scope():
            CT, CT_b = P.sb([128, 2, 257], F32, "CT")
            CTb, CTb_b = P.sb([128, 2, 257], BF16, "CTb")
            kTg = RR([P.sb([128, 2, 1024], BF16, f"kTg{i}") for i in range(2)])
            qTg = RR([P.sb([128, 2, 1024], BF16, f"qTg{i}") for i in range(2)])
            ktmg = RR([P.sb([128, 8, 256], BF16, f"ktmg{i}") for i in range(2)])
            vtmg = RR([P.sb([128, 8, 257], BF16, f"vtmg{i}") for i in range(2)])
            LFr = RR([P.sb([128, 128], F32, f"LF{i}") for i in range(4)])
            Bsr = RR([P.sb([128, 129], F32, f"Bs{i}") for i in range(4)])
            rcr = RR([P.sb([128, 8], F32, f"rc{i}") for i in range(8)])
            DTr = RR([P.sb([128, 128], F32, f"DT{i}") for i in range(4)])
            ATr = RR([P.sb([128, 128], F32, f"AT{i}") for i in range(4)])
            Ebr = RR([P.sb([128, 128], F32, f"Eb{i}") for i in range(4)])
            vwr = RR([P.sb([128, 257], BF16, f"vw{i}") for i in range(4)])
            STr = RR([P.sb([128, 128], BF16, f"ST{i}") for i in range(4)])
            qsr = RR([P.sb([128, 2, 128], BF16, f"qs{i}") for i in range(4)])
            hchr = RR([P.sb([128, 256], F32, f"hch{i}") for i in range(4)])
            hflr = RR([P.sb([128, 256], F32, f"hfl{i}") for i in range(4)])
            hsr = RR([P.sb([128, 256], F32, f"hs{i}") for i in range(4)])
            hnr = RR([P.sb([128, 256], F32, f"hn{i}") for i in range(4)])
            hnTr = RR([P.sb([128, 2, 128], F32, f"hnT{i}") for i in range(4)])
            sqh, sqh_b = P.sb([128, 256], BF16, "sqh")
            for (vt, vt_b) in vtmg.items:
                P.memset("pool", vt[:], 1.0, [vt_b])
            dq = RR(["sp"])
            NHD = int(os.environ.get("SCAN_NH", "4"))
            NGRP = int(os.environ.get("SCAN_NG", "8"))
            onesf = cs[:, 7, :]
            groups = [(0, 2)] + [(2 + 8 * g, 8) for g in range(NGRP)]
            for hd in range(NHD):
                for d_ in range(2):
                    P.memset("dve", CT[:], 0.0, [CT_b])
                    P.memset("pool", CTb[:], 0.0, [CTb_b])
                    Umat = cs[:, 1, :] if d_ == 0 else cs[:, 2, :]
                    maskT = cs[:, 3, :] if d_ == 0 else cs[:, 4, :]
                    lc = 127 if d_ == 0 else 0
                    gi = d_ * 8 + hd
                    fi = d_ * 8 + 4 + hd
                    gorder = groups if d_ == 0 else [groups[0]] + groups[1:][::-1]
                    seq = []
                    for (j0, ng) in gorder:
                        jjs = list(range(ng)) if d_ == 0 else list(range(ng))[::-1]
                        for jj in jjs:
                            seq.append((j0, ng, jj))
                    gstate = {}

                    def load_group(j0, ng):
                        latent = j0 >= 2
                        t0 = j0 * 128
                        ntok = ng * 128
                        kt_, kt_b = ktmg.next()
                        vt_, vt_b = vtmg.next()
                        P.dma(dq.next(), kt_[:, 0:ng, :], MK[t0:t0 + ntok, hd * 256:(hd + 1) * 256].rearrange("(i p) d -> p i d", p=128),
                              reads=[MK_b], writes=[kt_b], lane_buf=kt_b)
                        P.dma(dq.next(), vt_[:, 0:ng, 0:256], MV[t0:t0 + ntok, hd * 256:(hd + 1) * 256].rearrange("(i p) d -> p i d", p=128),
                              reads=[MV_b], writes=[vt_b], lane_buf=vt_b)
                        g = dict(kt=(kt_, kt_b), vt=(vt_, vt_b))
                        if latent:
                            kT_, kT_b = kTg.next()
                            qT_, qT_b = qTg.next()
                            P.dma(dq.next(), kT_[:, :, 0:ntok], MKT[hd * 256:(hd + 1) * 256, t0:t0 + ntok].rearrange("(c p) t -> p c t", p=128),
                                  reads=[MKT_b], writes=[kT_b], lane_buf=kT_b)
                            P.dma(dq.next(), qT_[:, :, 0:ntok], MQT[hd * 256:(hd + 1) * 256, t0 - TC:t0 - TC + ntok].rearrange("(c p) t -> p c t", p=128),
                                  reads=[MQT_b], writes=[qT_b], lane_buf=qT_b)
                            g["kT"] = (kT_, kT_b)
                            g["qT"] = (qT_, qT_b)
                        return g

                    def get_group(j0, ng):
                        if j0 not in gstate:
                            gstate.clear()
                            gstate[j0] = load_group(j0, ng)
                        return gstate[j0]

                    def prepA(j0, ng, jj):
                        j = j0 + jj
                        lfc = gtm[:, j, fi:fi + 1]
                        LF, LF_b = LFr.next()
                        P.ts("pool", LF[:], onesf, lfc, None, ALU.mult, None, [cs_b, gtm_b], [LF_b])
                        pB, pB_b = psr.next()
                        P.mm(pB[:, 0:128], LF[:], Umat, True, True, [LF_b, cs_b], [pB_b])
                        P.mm(pB[:, 128:129], Umat, lfc, True, True, [cs_b, gtm_b], [pB_b])
                        return dict(j0=j0, ng=ng, j=j, jj=jj, latent=j0 >= 2, pB=(pB, pB_b), g=get_group(j0, ng))

                    def prepB(c):
                        j = c["j"]
                        latent = c["latent"]
                        pB, pB_b = c["pB"]
                        igc = gtm[:, j, gi:gi + 1]
                        Bs, Bs_b = Bsr.next()
                        P.cp("act", Bs[:], pB[:, 0:129], [pB_b], [Bs_b])
                        rc, rc_b = rcr.next()
                        P.tt("dve", rc[:, 0:1], igc, Bs[:, 128:129], ALU.subtract, [gtm_b, Bs_b], [rc_b])
                        Eb, Eb_b = Ebr.next()
                        P.act(Eb[:], Bs[:, 0:128], AF.Exp, [Bs_b], [Eb_b])
                        if latent:
                            DT, DT_b = DTr.next()
                            AT, AT_b = ATr.next()
                            P.stt("dve", DT[:], Bs[:, 0:128], rc[:, 0:1], maskT, ALU.add, ALU.add, [Bs_b, rc_b, cs_b], [DT_b])
                            c["AT"] = (AT, AT_b)
                        P.act(rc[:, 1:2], rc[:, 0:1], AF.Exp, [rc_b, Bs_b], [rc_b], bias=Bs[:, lc:lc + 1])
                        if latent:
                            P.act(AT[:], DT[:], AF.Exp, [DT_b], [AT_b])
                        c["rc"] = (rc, rc_b)
                        c["Eb"] = (Eb, Eb_b)

                    def prepC(c):
                        jj = c["jj"]
                        g = c["g"]
                        vt_, vt_b = g["vt"]
                        rc, rc_b = c["rc"]
                        Eb, Eb_b = c["Eb"]
                        vw, vw_b = vwr.next()
                        P.ts("dve", vw[:], vt_[:, jj, :], rc[:, 1:2], None, ALU.mult, None, [vt_b, rc_b], [vw_b])
                        c["vw"] = (vw, vw_b)
                        if c["latent"]:
                            kT_, kT_b = g["kT"]
                            qT_, qT_b = g["qT"]
                            AT, AT_b = c["AT"]
                            pG, pG_b = psr.next()
                            for cc in range(2):
                                P.mm(pG[:, 0:128], kT_[:, cc, jj * 128:(jj + 1) * 128], qT_[:, cc, jj * 128:(jj + 1) * 128], cc == 0, cc == 1,
                                     [kT_b, qT_b], [pG_b])
                            qs, qs_b = qsr.next()
                            for cc in range(2):
                                P.tt("pool", qs[:, cc, :], qT_[:, cc, jj * 128:(jj + 1) * 128], Eb[:], ALU.mult, [qT_b, Eb_b], [qs_b])
                            ST, ST_b = STr.next()
                            P.tt("dve", ST[:], pG[:, 0:128], AT[:], ALU.mult, [pG_b, AT_b], [ST_b])
                            c["ST"] = (ST, ST_b)
                            c["qs"] = (qs, qs_b)

                    def main(c):
                        j = c["j"]
                        jj = c["jj"]
                        tq = (j - 2) * 128
                        kt_, kt_b = c["g"]["kt"]
                        vt_, vt_b = c["g"]["vt"]
                        rc, rc_b = c["rc"]
                        Eb, Eb_b = c["Eb"]
                        vw, vw_b = c["vw"]
                        pUs = []
                        for cc in range(2):
                            pU, pU_b = psr.next()
                            P.mm(pU[:, 0:257], kt_[:, jj, cc * 128:(cc + 1) * 128], vw[:], True, True, [kt_b, vw_b], [pU_b])
                            pUs.append((pU, pU_b))
                        if c["latent"]:
                            ST, ST_b = c["ST"]
                            qs, qs_b = c["qs"]
                            pN, pN_b = psr.next()
                            P.mm(pN[:, 0:257], ST[:], vt_[:, jj, :], True, False, [ST_b, vt_b], [pN_b])
                            P.mm(pN[:, 0:257], qs[:, 0, :], CTb[:, 0, :], False, False, [qs_b, CTb_b], [pN_b])
                            P.mm(pN[:, 0:257], qs[:, 1, :], CTb[:, 1, :], False, True, [qs_b, CTb_b], [pN_b])
                        for cc in range(2):
                            pU, pU_b = pUs[cc]
                            P.stt("dve", CT[:, cc, :], CT[:, cc, :], Eb[:, lc:lc + 1], pU[:, 0:257], ALU.mult, ALU.add, [CT_b, Eb_b, pU_b], [CT_b])
                        P.cp("act", CTb[:], CT[:], [CT_b], [CTb_b])
                        if c["latent"]:
                            P.ts("dve", rc[:, 2:3], pN[:, 256:257], -1.0, None, ALU.mult, None, [pN_b], [rc_b])
                            P.tt("dve", rc[:, 3:4], rc[:, 2:3], pN[:, 256:257], ALU.max, [rc_b, pN_b], [rc_b])
                            P.ts("dve", rc[:, 3:4], rc[:, 3:4], 1.0, None, ALU.max, None, [rc_b], [rc_b])
                            P.op("dve", (lambda e, a=rc[:, 4:5], b=rc[:, 3:4]: e.reciprocal(out=a, in_=b)), [rc_b], [rc_b])
                            hch, hch_b = hchr.next()
                            P.ts("dve", hch[:], pN[:, 0:256], rc[:, 4:5], None, ALU.mult, None, [pN_b, rc_b], [hch_b])
                            if d_ == 0:
                                P.dma(dq.next(), HF[tq:tq + 128, hd * 256:(hd + 1) * 256], hch[:], reads=[hch_b], writes=[HF_b], lane_buf=hch_b)
                            else:
                                hfl, hfl_b = hflr.next()
                                P.dma(dq.next(), hfl[:], HF[tq:tq + 128, hd * 256:(hd + 1) * 256], reads=[HF_b], writes=[hfl_b], lane_buf=hfl_b)
                                hs_, hs_b = hsr.next()
                                P.tt("pool", hs_[:], hch[:], hfl[:], ALU.add, [hch_b, hfl_b], [hs_b])
                                P.memset("pool", rc[:, 5:6], 0.0, [rc_b])
                                c["hs"] = (hs_, hs_b)

                    def ro1(c):
                        if "hs" not in c:
                            return
                        rc, rc_b = c["rc"]
                        hs_, hs_b = c["hs"]
                        P.act(sqh[:], hs_[:], AF.Square, [hs_b, rc_b], [sqh_b, rc_b], accum_out=rc[:, 5:6])
                        P.act(rc[:, 6:7], rc[:, 5:6], AF.Ln, [rc_b], [rc_b], scale=1.0 / 256.0, bias=EPS)
                        P.act(rc[:, 7:8], rc[:, 6:7], AF.Exp, [rc_b], [rc_b], scale=-0.5)
                        hn, hn_b = hnr.next()
                        P.act(hn[:], hs_[:], AF.Copy, [hs_b, rc_b], [hn_b], scale=rc[:, 7:8])
                        c["hn"] = (hn, hn_b)

                    def ro2(c):
                        if "hn" not in c:
                            return
                        hn, hn_b = c["hn"]
                        pH, pH_b = psr.next()
                        for cc in range(2):
                            P.tr(pH[:, cc * 128:(cc + 1) * 128], hn[:, cc * 128:(cc + 1) * 128], ident, [hn_b, cs_b], [pH_b])
                        c["pH"] = (pH, pH_b)

                    def ro3(c):
                        if "pH" not in c:
                            return
                        tq = (c["j"] - 2) * 128
                        pH, pH_b = c["pH"]
                        hnT, hnT_b = hnTr.next()
                        P.cp("act", hnT[:].rearrange("p c t -> p (c t)"), pH[:, 0:256], [pH_b], [hnT_b])
                        P.dma(dq.next(), HNT[hd * 256:(hd + 1) * 256, tq:tq + 128].rearrange("(c p) t -> p c t", p=128), hnT[:],
                              reads=[hnT_b], writes=[HNT_b], lane_buf=hnT_b)

                    n_ = len(seq)
                    tiles = {}
                    for it in range(-3, n_ + 3):
                        if 0 <= it < n_:
                            main(tiles[it])
                        if 0 <= it - 1 < n_:
                            ro1(tiles[it - 1])
                        if 0 <= it - 2 < n_:
                            ro2(tiles[it - 2])
                        if 0 <= it - 3 < n_:
                            ro3(tiles.pop(it - 3))
                        if 0 <= it + 1 < n_:
                            prepC(tiles[it + 1])
                        if 0 <= it + 2 < n_:
                            prepB(tiles[it + 2])
                        if 0 <= it + 3 < n_:
                            tiles[it + 3] = prepA(*seq[it + 3])

        def stage_merge():
          with P.scope():
            stg = RR([P.sb([128, 8, 256], F32, f"mstg{i}") for i in range(2)])
            wout, wout_b = P.sb([128, 8, D], BF16, "wout")
            wr, wr_b = P.sb([128, 8, 16], F32, "wr")
            ldr = [RR([P.sb([128, 512], BF16 if n in (1, 2, 3) else F32, f"ld{n}{i}") for i in range(4)]) for n in range(6)]
            t1r = RR([P.sb([128, 512], F32, f"mt1{i}") for i in range(2)])
            t2r = RR([P.sb([128, 512], F32, f"mt2{i}") for i in range(2)])
            mTr = RR([P.sb([128, 8, 512], BF16, f"mT{i}") for i in range(2)])
            xtr = RR([P.sb([128, D], F32, f"mx{i}") for i in range(2)])
            x1r = RR([P.sb([128, D], F32, f"mx1{i}") for i in range(2)])
            tmpr = RR([P.sb([128, D], F32, f"mtmp{i}") for i in range(2)])
            h2r = RR([P.sb([128, D], F32, f"mh2{i}") for i in range(2)])
            h2br = RR([P.sb([128, D], BF16, f"mh2b{i}") for i in range(2)])
            sqm, sqm_b = P.sb([128, D], BF16, "sqm")
            ssr = RR([P.sb([128, 8], F32, f"mss{i}") for i in range(2)])
            h2Tr = RR([P.sb([128, 8, 128], F32, f"h2T{i}") for i in range(2)])
            lgr = RR([P.sb([128, 40], F32, f"lg{i}") for i in range(2)])
            dq = RR(["sp"])
            for j in range(4):
                st, st_b = stg.next()
                P.dma(dq.next(), st[:], w_out[:, j * 256:(j + 1) * 256].rearrange("(c p) n -> p c n", p=128), writes=[st_b], lane_buf=st_b)
                P.cp("act" if j % 2 else "dve", wout[:, :, j * 256:(j + 1) * 256], st[:], [st_b], [wout_b])
            P.dma("sp", wr[:], w_router, writes=[wr_b], lane_buf=wr_b)
            srcs = [(OT, OT_b), (GA, GA_b), (GM, GM_b), (ZS, ZS_b), (HNT, HNT_b), (XCT, XCT_b)]
            NB = int(os.environ.get("MRG_NB", "16"))
            def blk_gen(blk):
                tq0 = blk * 512
                mT, mT_b = mTr.next()
                for c in range(8):
                    lds = []
                    for n, (src, src_b) in enumerate(srcs):
                        t_, t_b = ldr[n].next()
                        P.dma(dq.next(), t_[:], src[c * 128:(c + 1) * 128, tq0:tq0 + 512], reads=[src_b], writes=[t_b], lane_buf=t_b)
                        lds.append((t_, t_b))
                    (ot, ot_b), (ga, ga_b), (gm_, gm_b), (zs, zs_b), (hnt, hnt_b), (xct, xct_b) = lds
                    t1, t1_b = t1r.next()
                    t2, t2_b = t2r.next()
                    P.act(t1[:], xct[:], AF.Copy, [xct_b, colt_b], [t1_b], scale=colt[:, c, 3:4])
                    P.stt("dve", t2[:], hnt[:], colt[:, c, 2:3], t1[:], ALU.mult, ALU.add, [hnt_b, colt_b, t1_b], [t2_b])
                    P.tt("pool", t1[:], t2[:], zs[:], ALU.mult, [t2_b, zs_b], [t1_b])
                    P.tt("dve", t2[:], ot[:], ga[:], ALU.mult, [ot_b, ga_b], [t2_b])
                    P.tt("pool", t1[:], t1[:], gm_[:], ALU.mult, [t1_b, gm_b], [t1_b])
                    P.tt("dve", mT[:, c, :], t2[:], t1[:], ALU.add, [t2_b, t1_b], [mT_b])
                yield
                for i in range(4):
                    tok = tq0 + i * 128
                    jt = tok // 128
                    xt, xt_b = xtr.next()
                    P.dma(dq.next(), xt[:], xcat[TC + tok:TC + tok + 128, :], writes=[xt_b], lane_buf=xt_b)
                    tmp, tmp_b = tmpr.next()
                    for hh in range(2):
                        pt, pt_b = psr.next()
                        for k in range(8):
                            P.mm(pt[:, :], mT[:, k, i * 128:(i + 1) * 128], wout[:, k, hh * 512:(hh + 1) * 512], k == 0, k == 7, [mT_b, wout_b], [pt_b])
                        P.tt("dve", tmp[:, hh * 512:(hh + 1) * 512], pt[:, :], bc[:, 0, hh * 512:(hh + 1) * 512], ALU.mult, [pt_b, bc_b], [tmp_b])
                    x1t, x1t_b = x1r.next()
                    P.tt("dve", x1t[:], tmp[:], xt[:], ALU.add, [tmp_b, xt_b], [x1t_b])
                    P.dma(dq.next(), X1[tok:tok + 128, :], x1t[:], reads=[x1t_b], writes=[X1_b], lane_buf=x1t_b)
                    ss, ss_b = ssr.next()
                    P.memset("dve", ss[:], 0.0, [ss_b])
                    P.act(sqm[:], x1t[:], AF.Square, [x1t_b, ss_b], [sqm_b, ss_b], accum_out=ss[:, 0:1])
                    P.act(ss[:, 1:2], ss[:, 0:1], AF.Ln, [ss_b], [ss_b], scale=1.0 / D, bias=EPS)
                    P.act(ss[:, 2:3], ss[:, 1:2], AF.Exp, [ss_b], [ss_b], scale=-0.5)
                    h2, h2_b = h2r.next()
                    P.stt("dve", h2[:], x1t[:], ss[:, 2:3], bc[:, 1, :], ALU.mult, ALU.mult, [x1t_b, ss_b, bc_b], [h2_b])
                    P.tt("pool", h2[:], h2[:], bc[:, 2, :], ALU.add, [h2_b, bc_b], [h2_b])
                    h2b, h2b_b = h2br.next()
                    P.cp("act", h2b[:], h2[:], [h2_b], [h2b_b])
                    P.dma(dq.next(), H2[tok:tok + 128, :], h2b[:], reads=[h2b_b], writes=[H2_b], lane_buf=h2b_b)
                    h2T, h2T_b = h2Tr.next()
                    for hh in range(2):
                        pt, pt_b = psr.next()
                        for c4 in range(4):
                            c = hh * 4 + c4
                            P.tr(pt[:, c4 * 128:(c4 + 1) * 128], h2[:, c * 128:(c + 1) * 128], ident, [h2_b, cs_b], [pt_b])
                        P.cp("act" if hh else "dve", h2T[:, hh * 4:(hh + 1) * 4, :].rearrange("p c t -> p (c t)"), pt[:, :], [pt_b], [h2T_b])
                    pt, pt_b = psr.next()
                    for c in range(8):
                        P.mm(pt[:, 0:16], h2T[:, c, :], wr[:, c, :], c == 0, c == 7, [h2T_b, wr_b], [pt_b])
                    lg, lg_b = lgr.next()
                    P.cp("dve", lg[:, 0:16], pt[:, 0:16], [pt_b], [lg_b])
                    P.op("dve", (lambda e, a=lg[:, 32:33], b=lg[:, 0:16]: e.tensor_reduce(out=a, in_=b, axis=AX.X, op=ALU.max)), [lg_b], [lg_b])
                    P.ts("dve", lg[:, 33:34], lg[:, 32:33], -1.0, None, ALU.mult, None, [lg_b], [lg_b])
                    P.memset("dve", lg[:, 34:35], 0.0, [lg_b])
                    P.act(lg[:, 16:32], lg[:, 0:16], AF.Exp, [lg_b], [lg_b], bias=lg[:, 33:34], accum_out=lg[:, 34:35])
                    P.op("dve", (lambda e, a=lg[:, 35:36], b=lg[:, 34:35]: e.reciprocal(out=a, in_=b)), [lg_b], [lg_b])
                    P.ts("dve", aff[:, jt, :], lg[:, 16:32], lg[:, 35:36], None, ALU.mult, None, [lg_b], [aff_b])
            gens = [blk_gen(b_) for b_ in range(NB)]
            if gens:
                next(gens[0])
            for gi_ in range(len(gens)):
                if gi_ + 1 < len(gens):
                    next(gens[gi_ + 1])
                for _ in gens[gi_]:
                    pass
            if "DBG_aff" in P.debug:
                da_, da_b = P.dram("DBG_aff", [128, 64 * 16], F32)
                finals.append(P.dma("sp", da_, aff[:].rearrange("p j g -> p (j g)"), reads=[aff_b], lane_buf=aff_b))

        def stage_route():
          with P.scope():
            lo, lo_b = P.sb([128, 16], F32, "lo")
            hi, hi_b = P.sb([128, 16], F32, "hi")
            mid, mid_b = P.sb([128, 16], F32, "mid")
            cntp, cntp_b = P.sb([128, 16], F32, "cntp")
            ge, ge_b = P.sb([128, 16], F32, "ge")
            ta, ta_b = P.sb([128, 16], F32, "ta")
            tb, tb_b = P.sb([128, 16], F32, "tb")
            eoff, eoff_b = P.sb([128, 16], F32, "eoff")
            cmpt, cmpt_b = P.sb([128, 64, 16], F32, "cmp")
            maskf, maskf_b = P.sb([128, 64, 16], F32, "maskf")
            maskb, maskb_b = P.sb([128, 1024], BF16, "maskb")
            pwS, pwS_b = P.sb([128, 64, 16], F32, "pwS")
            totS, totS_b = P.sb([128, 64, 16], F32, "totS")
            scA, scA_b = P.sb([128, 64, 16], F32, "scA")
            scB, scB_b = P.sb([128, 64, 16], F32, "scB")
            onesf = cs[:, 7, :]
            P.memset("dve", lo[:], 0.0, [lo_b])
            P.memset("dve", hi[:], 2.0, [hi_b])
            for e_ in range(16):
                P.memset("pool", eoff[:, e_:e_ + 1], float(e_ * CAP), [eoff_b])
            affv = aff[:]
            for it in range(int(os.environ.get("ROUTE_ITERS", "40"))):
                P.tt("dve", mid[:], lo[:], hi[:], ALU.add, [lo_b, hi_b], [mid_b])
                P.ts("dve", mid[:], mid[:], 0.5, None, ALU.mult, None, [mid_b], [mid_b])
                P.tt("dve", cmpt[:], affv, mid[:].unsqueeze(1).to_broadcast([128, 64, 16]), ALU.is_ge, [aff_b, mid_b], [cmpt_b])
                P.op("dve", (lambda e, a=cntp[:], b=cmpt[:].rearrange("p j g -> p g j"): e.tensor_reduce(out=a, in_=b, axis=AX.X, op=ALU.add)),
                     [cmpt_b], [cntp_b])
                pt, pt_b = psr.next()
                P.mm(pt[:, 0:16], onesf, cntp[:], True, True, [cs_b, cntp_b], [pt_b])
                P.ts("dve", ge[:], pt[:, 0:16], float(CAP), None, ALU.is_ge, None, [pt_b], [ge_b])
                P.tt("dve", ta[:], ge[:], mid[:], ALU.mult, [ge_b, mid_b], [ta_b])
                P.tt("dve", lo[:], lo[:], ta[:], ALU.max, [lo_b, ta_b], [lo_b])
                P.stt("dve", tb[:], ge[:], 4.0, mid[:], ALU.mult, ALU.add, [ge_b, mid_b], [tb_b])
                P.tt("dve", hi[:], hi[:], tb[:], ALU.min, [hi_b, tb_b], [hi_b])
            P.tt("dve", maskf[:], affv, lo[:].unsqueeze(1).to_broadcast([128, 64, 16]), ALU.is_ge, [aff_b, lo_b], [maskf_b])
            P.cp("dve", maskb[:], maskf[:].rearrange("p j g -> p (j g)"), [maskf_b], [maskb_b])
            for hh in range(2):
                pt, pt_b = psr.next()
                P.mm(pt[:, :], csb[:, 5, :], maskb[:, hh * 512:(hh + 1) * 512], True, True, [csb_b, maskb_b], [pt_b])
                P.cp("dve", pwS[:].rearrange("p j g -> p (j g)")[:, hh * 512:(hh + 1) * 512], pt[:, :], [pt_b], [pwS_b])
                pt, pt_b = psr.next()
                P.mm(pt[:, :], onesb, maskb[:, hh * 512:(hh + 1) * 512], True, True, [csb_b, maskb_b], [pt_b])
                P.cp("act", totS[:].rearrange("p j g -> p (j g)")[:, hh * 512:(hh + 1) * 512], pt[:, :], [pt_b], [totS_b])
            P.cp("dve", scA[:], totS[:], [totS_b], [scA_b])
            A, A_b, B, B_b = scA, scA_b, scB, scB_b
            for sft in (1, 2, 4, 8, 16, 32):
                P.cp("dve", B[:, 0:sft, :], A[:, 0:sft, :], [A_b], [B_b])
                P.tt("dve", B[:, sft:64, :], A[:, sft:64, :], A[:, 0:64 - sft, :], ALU.add, [A_b], [B_b])
                A, A_b, B, B_b = B, B_b, A, A_b
            P.tt("dve", B[:], A[:], totS[:], ALU.subtract, [A_b, totS_b], [B_b])
            P.tt("dve", B[:], B[:], pwS[:], ALU.add, [B_b, pwS_b], [B_b])
            P.ts("dve", A[:], B[:], float(CAP), None, ALU.is_lt, None, [B_b], [A_b])
            P.tt("dve", A[:], A[:], maskf[:], ALU.mult, [A_b, maskf_b], [A_b])
            P.tt("dve", gmv[:], affv, A[:], ALU.mult, [aff_b, A_b], [gmv_b])
            P.tt("dve", B[:], B[:], eoff[:].unsqueeze(1).to_broadcast([128, 64, 16]), ALU.add, [B_b, eoff_b], [B_b])
            P.stt("dve", B[:], B[:], -1.0e6, A[:], ALU.add, ALU.mult, [B_b, A_b], [B_b])
            P.ts("dve", B[:], B[:], 1.0e6, None, ALU.add, None, [B_b], [B_b])
            P.cp("dve", posi[:], B[:].rearrange("p j g -> p (j g)"), [B_b], [posi_b])
            if "DBG_pos" in P.debug:
                dp_, dp_b = P.dram("DBG_pos", [128, 64 * 16], I32)
                finals.append(P.dma("sp", dp_, posi[:], reads=[posi_b], lane_buf=posi_b))
                dg2_, dg2_b = P.dram("DBG_gmv", [128, 64 * 16], F32)
                finals.append(P.dma("sp", dg2_, gmv[:].rearrange("p j g -> p (j g)"), reads=[gmv_b], lane_buf=gmv_b))

        def stage_scatter():
          with P.scope():
            h2lr = RR([P.sb([128, D], BF16, f"h2l{i}") for i in range(3)])
            for j in range(64):
                h2l, h2l_b = h2lr.next()
                P.dma("sp", h2l[:], H2[j * 128:(j + 1) * 128, :], reads=[H2_b], writes=[h2l_b], lane_buf=h2l_b)
                for e_ in range(NEXP):
                    P.dma("pool", None, None, reads=[h2l_b, posi_b], lane_buf=h2l_b,
                          fn=(lambda e, o=XS[:, :], off=posi[:, j * 16 + e_:j * 16 + e_ + 1], i_=h2l[:, :]: e.indirect_dma_start(
                              out=o, out_offset=bass.IndirectOffsetOnAxis(ap=off, axis=0), in_=i_, in_offset=None,
                              bounds_check=P.pool_reg, oob_is_err=False)))

        def stage_experts():
          with P.scope():
            identb = csb[:, 0, :]
            xsr = RR([P.sb([128, D], BF16, f"xs{i}") for i in range(3)])
            xsT, xsT_b = P.sb([128, 8, CAP], BF16, "xsT")
            wtr = [RR([P.sb([128, 8, D], BF16, f"we{i}_{k}") for k in range(2)]) for i in range(3)]
            actT, actT_b = P.sb([128, 8, CAP], BF16, "actT")
            sar = RR([P.sb([128, 512], F32, f"sa{i}") for i in range(2)])
            ysr = RR([P.sb([128, D], F32, f"ys{i}") for i in range(2)])
            NE_ = int(os.environ.get("MOE_NE", "16"))

            def load_w(e_):
                res = []
                for wi, wsrc in enumerate((w_eg, w_eu, w_ed)):
                    wt, wt_b = wtr[wi].next()
                    for j in range(4):
                        P.dma("pool", wt[:, :, j * 256:(j + 1) * 256], wsrc[e_, :, j * 256:(j + 1) * 256].rearrange("(c p) n -> p c n", p=128),
                              writes=[wt_b], lane_buf=wt_b)
                    res.append((wt, wt_b))
                return res
            wnext = load_w(0)
            for e_ in range(NE_):
                (wg_, wg_b), (wu_, wu_b), (wd_, wd_b) = wnext
                if e_ + 1 < NE_:
                    wnext = load_w(e_ + 1)
                for i in range(8):
                    xs, xs_b = xsr.next()
                    P.dma("sp", xs[:], XS[e_ * CAP + i * 128:e_ * CAP + (i + 1) * 128, :], reads=[XS_b], writes=[xs_b], lane_buf=xs_b)
                    for k in range(8):
                        P.tr(psbf[:, k * 128:(k + 1) * 128], xs[:, k * 128:(k + 1) * 128], identb, [xs_b, csb_b], [psbf_b])
                    P.cp("act" if i % 2 else "dve", xsT[:, :, i * 128:(i + 1) * 128], psbf[:, :].rearrange("p (k t) -> p k t", t=128), [psbf_b], [xsT_b])
                for f in range(8):
                    for hh in range(2):
                        pa, pa_b = psr.next()
                        pu, pu_b = psr.next()
                        for k in range(8):
                            P.mm(pa[:, :], wg_[:, k, f * 128:(f + 1) * 128], xsT[:, k, hh * 512:(hh + 1) * 512], k == 0, k == 7, [wg_b, xsT_b], [pa_b])
                        for k in range(8):
                            P.mm(pu[:, :], wu_[:, k, f * 128:(f + 1) * 128], xsT[:, k, hh * 512:(hh + 1) * 512], k == 0, k == 7, [wu_b, xsT_b], [pu_b])
                        sa, sa_b = sar.next()
                        P.act(sa[:], pa[:, :], AF.Silu, [pa_b], [sa_b])
                        P.tt("dve", actT[:, f, hh * 512:(hh + 1) * 512], sa[:], pu[:, :], ALU.mult, [sa_b, pu_b], [actT_b])
                for i in range(8):
                    ys, ys_b = ysr.next()
                    for hh in range(2):
                        py, py_b = psr.next()
                        for f in range(8):
                            P.mm(py[:, :], actT[:, f, i * 128:(i + 1) * 128], wd_[:, f, hh * 512:(hh + 1) * 512], f == 0, f == 7, [actT_b, wd_b], [py_b])
                        P.tt("dve", ys[:, hh * 512:(hh + 1) * 512], py[:, :], bc[:, 3, hh * 512:(hh + 1) * 512], ALU.mult, [py_b, bc_b], [ys_b])
                    P.dma("sp", YY[e_ * CAP + i * 128:e_ * CAP + (i + 1) * 128, :], ys[:], reads=[ys_b], lane_buf=ys_b)

        def stage_final():
          with P.scope():
            ygr = RR([P.sb([128, D], F32, f"yg{i}") for i in range(8)])
            x1r = RR([P.sb([128, D], F32, f"fx1{i}") for i in range(2)])
            acr = RR([P.sb([128, D], F32, f"fac{i}") for i in range(2)])
            tmr = RR([P.sb([128, D], F32, f"ftm{i}") for i in range(2)])
            outr = RR([P.sb([128, D], F32, f"fo{i}") for i in range(2)])
            sqf, sqf_b = P.sb([128, D], BF16, "sqf")
            ssr = RR([P.sb([128, 8], F32, f"fss{i}") for i in range(2)])
            for (yg_, yg_b) in ygr.items:
                P.memset("pool", yg_[:], 0.0, [yg_b])
            NJ = int(os.environ.get("FIN_NJ", "64"))
            for j in range(NJ):
                x1t, x1t_b = x1r.next()
                P.dma("sp", x1t[:], X1[j * 128:(j + 1) * 128, :], reads=[X1_b], writes=[x1t_b], lane_buf=x1t_b)
                tm, tm_b = tmr.next()
                for e_ in range(NEXP):
                    yg_, yg_b = ygr.next()
                    P.dma("pool", None, None, reads=[posi_b], writes=[yg_b], lane_buf=yg_b,
                          fn=(lambda e, o=yg_[:, :], off=posi[:, j * 16 + e_:j * 16 + e_ + 1], i_=YY[:, :]: e.indirect_dma_start(
                              out=o, out_offset=None, in_=i_, in_offset=bass.IndirectOffsetOnAxis(ap=off, axis=0),
                              bounds_check=P.pool_reg, oob_is_err=False)))
                    if e_ == 0:
                        P.stt("dve", tm[:], yg_[:], gmv[:, j, e_:e_ + 1], x1t[:], ALU.mult, ALU.add, [yg_b, gmv_b, x1t_b], [tm_b])
                    else:
                        P.stt("dve", tm[:], yg_[:], gmv[:, j, e_:e_ + 1], tm[:], ALU.mult, ALU.add, [yg_b, gmv_b, tm_b], [tm_b])
                ss, ss_b = ssr.next()
                P.memset("dve", ss[:], 0.0, [ss_b])
                P.act(sqf[:], tm[:], AF.Square, [tm_b, ss_b], [sqf_b, ss_b], accum_out=ss[:, 0:1])
                P.act(ss[:, 1:2], ss[:, 0:1], AF.Ln, [ss_b], [ss_b], scale=1.0 / D, bias=EPS)
                P.act(ss[:, 2:3], ss[:, 1:2], AF.Exp, [ss_b], [ss_b], scale=-0.5)
                ot_, ot_b = outr.next()
                P.stt("dve", ot_[:], tm[:], ss[:, 2:3], bc[:, 4, :], ALU.mult, ALU.mult, [tm_b, ss_b, bc_b], [ot_b])
                finals.append(P.dma("sp", out[j * 128:(j + 1) * 128, :], ot_[:], reads=[ot_b], lane_buf=ot_b))

        if "2" in ST:
            stage2()
        if "attn" in ST:
            stage_attn()
        if "scan" in ST:
            stage_scan()
        if "merge" in ST:
            stage_merge()
        if "moe" in ST:
            stage_route()
            stage_scatter()
            stage_experts()
        if "final" in ST:
            stage_final()

        for nm, (ap_, b_) in {"XMT": (XMT, XMT_b), "ZS": (ZS, ZS_b), "GA": (GA, GA_b), "GM": (GM, GM_b), "QN": (QN, QN_b),
                              "QR": (QR, QR_b), "KN": (KN, KN_b), "KR": (KR, KR_b), "VV": (VV, VV_b)}.items():
            if nm in P.debug and b_.last_w is not None:
                pass
        last = P.barrier()
        finals.append(last)
        P.emit(final_wait_ops=finals)
    return nc, P


def _chunks(v, n):
    return np.ascontiguousarray(np.asarray(v, np.float32).reshape(n, 128).T)


def prep_inputs(x, c, ctx, c_ctx, w_mod, b_mod, norm1, w_in, q_norm, w_uq, kv_norm, w_ukv, conv_w, conv_b,
                w_qblk, w_kblk, w_vblk, w_gate, b_gate, ml_norm, ml_skip, w_out, norm2, w_router,
                w_e_gate, w_e_up, w_e_down, final_norm):
    f = lambda a: np.asarray(a, np.float32)
    shared = {}
    shared["w_mod"] = np.ascontiguousarray(f(w_mod)[0])
    rowv = np.zeros((2, 8 * D), np.float32)
    rowv[0, :6 * D] = f(b_mod)[0]
    rowv[1, :6 * D] = f(b_mod)[0]
    rowv[0, 6 * D:7 * D] = f(norm2)[0]
    rowv[0, 7 * D:8 * D] = f(final_norm)
    shared["rowv"] = rowv
    colv = np.zeros((128, 8, 9), np.float32)
    colv[:, :, 0] = _chunks(f(norm1)[0], 8)
    colv[:, :, 1] = _chunks(f(conv_b)[0], 8)
    colv[:, :, 2] = _chunks(f(ml_norm)[0], 8)
    colv[:, :, 3] = _chunks(f(ml_skip)[0], 8)
    for j in range(5):
        colv[:, :, 4 + j] = _chunks(f(conv_w)[0, j], 8)
    shared["colv"] = colv
    qkn = np.zeros((128, 5), np.float32)
    qkn[:, 0:3] = _chunks(f(q_norm)[0], 3)
    qkn[:, 3:5] = _chunks(f(kv_norm)[0], 2)
    shared["qkn"] = qkn
    perm = np.concatenate([np.arange(16, 32), np.arange(0, 16), np.arange(48, 64), np.arange(32, 48)])
    wi = f(w_in)[0]
    shared["w_in"] = np.ascontiguousarray(np.concatenate([wi, wi[:, 640:704][:, perm]], axis=1))
    wq = f(w_uq)[0].reshape(384, 8, 192)
    shared["w_uq"] = np.ascontiguousarray(np.concatenate(
        [wq[:, :, :128].reshape(384, 1024), wq[:, :, 128:].reshape(384, 512), wq[:, :, 128:][:, :, perm].reshape(384, 512)], axis=1))
    wkv = f(w_ukv)[0].reshape(256, 8, 256)
    shared["w_ukv"] = np.ascontiguousarray(np.concatenate([wkv[:, :, :128].reshape(256, 1024), wkv[:, :, 128:].reshape(256, 1024)], axis=1))
    pos = np.arange(T)
    row = (pos // 64).astype(np.float32)
    col = (pos % 64).astype(np.float32)
    inv = (np.float32(10000.0) ** (-np.arange(16, dtype=np.float32) / np.float32(16))).astype(np.float32)
    ar = row[:, None] * inv
    ac = col[:, None] * inv
    cos64 = np.concatenate([np.cos(ar), np.cos(ar), np.cos(ac), np.cos(ac)], axis=1).T
    sin64 = np.concatenate([-np.sin(ar), np.sin(ar), -np.sin(ac), np.sin(ac)], axis=1).T
    ropet = np.zeros((2, 128, TA), np.float32)
    ropet[0, :, :TC] = 1.0
    ropet[0, 0:64, TC:] = cos64
    ropet[0, 64:128, TC:] = cos64
    ropet[1, 0:64, TC:] = sin64
    ropet[1, 64:128, TC:] = sin64
    shared["ropet"] = ropet
    cst = np.zeros((128, 8, 128), np.float32)
    i = np.arange(128)
    cst[:, 0, :] = np.eye(128)
    cst[:, 1, :] = (i[:, None] <= i[None, :])
    cst[:, 2, :] = (i[:, None] >= i[None, :])
    cst[:, 3, :] = np.where(i[:, None] <= i[None, :], 0.0, NEG)
    cst[:, 4, :] = np.where(i[:, None] >= i[None, :], 0.0, NEG)
    cst[:, 5, :] = (i[:, None] < i[None, :])
    cst[0, 6, :] = 1.0
    cst[:, 7, :] = 1.0
    shared["cst"] = cst
    wbd = np.zeros((128, 24, 128), np.float32)
    for wi_, wsrc in enumerate((w_qblk, w_kblk, w_vblk)):
        wb = f(wsrc)[0]
        for cc in range(8):
            for nn in range(32):
                wbd[nn * 4:(nn + 1) * 4, wi_ * 8 + cc, nn * 4:(nn + 1) * 4] = wb[cc * 32 + nn]
    shared["w_bd"] = wbd
    shared["w_gate"] = np.ascontiguousarray(f(w_gate)[0].reshape(24, 128, 16).transpose(1, 0, 2))
    shared["b_gate"] = np.ascontiguousarray(f(b_gate)[0].reshape(16, 1))
    shared["w_out"] = np.ascontiguousarray(f(w_out)[0])
    shared["w_router"] = np.ascontiguousarray(f(w_router)[0].reshape(8, 128, 16).transpose(1, 0, 2))
    shared["w_eg"] = np.ascontiguousarray(f(w_e_gate)[0])
    shared["w_eu"] = np.ascontiguousarray(f(w_e_up)[0])
    shared["w_ed"] = np.ascontiguousarray(f(w_e_down)[0])
    shared["tokid"] = np.ascontiguousarray((np.arange(64)[None, :] * 128 + np.arange(128)[:, None]).astype(np.int32))
    per_b = []
    for b in range(2):
        d = dict(shared)
        d["xcat"] = np.ascontiguousarray(np.concatenate([f(ctx)[b], f(x)[b]], axis=0))
        cv = np.zeros((128, 8, 2), np.float32)
        cv[:, :, 0] = _chunks(f(c)[b], 8)
        cv[:, :, 1] = _chunks(f(c_ctx), 8)
        d["cvec"] = cv
        per_b.append(d)
    return per_b


def kernel(**inputs):
    per_b = prep_inputs(**inputs)
    nc, P = build()
    in_maps = [per_b[0], per_b[1]]
    res = run_bass_kernel_spmd(nc, in_maps, core_ids=[0, 1])
    return np.stack([res.results[0]["out"], res.results[1]["out"]], axis=0).astype(np.float32)
```

```python
import os
import numpy as np
from contextlib import ExitStack, contextmanager
import concourse.bass as bass
import concourse.mybir as mybir
from concourse.bass_utils import run_bass_kernel_spmd

F32 = mybir.dt.float32
BF16 = mybir.dt.bfloat16
I32 = mybir.dt.int32
ALU = mybir.AluOpType
AF = mybir.ActivationFunctionType
AX = mybir.AxisListType

D = 1024
T = 8192
TC = 256
TA = T + TC
NEXP = 16
CAP = 1024
EPS = 1e-6
SEM_EPOCH = 30000
NEG = -1.0e30


class Buf:
    __slots__ = ("name", "last_w", "readers", "lane", "excl")

    def __init__(self, name, excl=False):
        self.name = name
        self.excl = excl
        self.last_w = None
        self.readers = []
        self.lane = None


class Op:
    __slots__ = ("eng", "fn", "deps", "is_dma", "lane", "lane_seq", "sig", "sig_idx", "idx")


class Prog:
    ENGS = ("pe", "act", "dve", "pool", "sp")

    def __init__(self, nc, stack, debug=()):
        self.nc = nc
        self.stack = stack
        self.cur = stack
        self.ops = []
        self.lanes = []
        self.n_tiles = 0
        self.epoch = None
        self.since = []
        self.debug = set(debug)
        self.free_lanes = []
        self.scope_lanes = [[]]

    def sb(self, shape, dtype, name=None):
        self.n_tiles += 1
        name = (name or "t") + f"_{self.n_tiles}"
        t = self.cur.enter_context(self.nc.sbuf_tensor(name, list(shape), dtype))
        return t, Buf(name)

    def ps(self, shape, dtype, name=None):
        self.n_tiles += 1
        name = (name or "p") + f"_{self.n_tiles}"
        t = self.cur.enter_context(self.nc.psum_tensor(name, list(shape), dtype))
        return t, Buf(name, excl=True)

    def dram(self, name, shape, dtype):
        kind = "ExternalOutput" if name in self.debug else "Internal"
        t = self.nc.dram_tensor(name, list(shape), dtype, kind=kind)
        return t.ap(), Buf(name)

    @contextmanager
    def scope(self):
        prev = self.cur
        st = ExitStack()
        self.cur = st
        self.scope_lanes.append([])
        try:
            yield
        finally:
            self.barrier()
            st.close()
            self.cur = prev
            self.free_lanes.extend(self.scope_lanes.pop())

    def _deps(self, op, reads, writes):
        ex = [r for r in reads if r.excl]
        if ex:
            reads = [r for r in reads if not r.excl]
            writes = list(writes) + [r for r in ex if r not in writes]
        deps = set()
        if self.epoch is not None:
            deps.add(self.epoch)
        for r in reads:
            if r.last_w is not None:
                deps.add(r.last_w)
        for w in writes:
            if w.last_w is not None:
                deps.add(w.last_w)
            for rd in w.readers:
                deps.add(rd)
        deps.discard(op)
        for r in reads:
            r.readers.append(op)
        for w in writes:
            w.last_w = op
            w.readers = []
        op.deps = deps

    def op(self, eng, fn, reads=(), writes=()):
        o = Op()
        o.eng = eng
        o.fn = fn
        o.is_dma = False
        o.lane = None
        o.lane_seq = 0
        o.sig = False
        o.idx = len(self.ops)
        self._deps(o, reads, writes)
        self.ops.append(o)
        self.since.append(o)
        return o

    def dma(self, queue, out, in_, reads=(), writes=(), lane_buf=None, fn=None, **kw):
        o = Op()
        o.eng = queue
        o.is_dma = True
        if lane_buf.lane is None:
            lane_buf.lane = {}
        if queue not in lane_buf.lane:
            fl = [l for l in self.free_lanes if l[0] == queue]
            if fl:
                self.free_lanes.remove(fl[0])
                lane_buf.lane[queue] = fl[0][1]
            else:
                lane_buf.lane[queue] = len(self.lanes)
                self.lanes.append([None, 0])
            self.scope_lanes[-1].append((queue, lane_buf.lane[queue]))
        o.lane = lane_buf.lane[queue]
        self.lanes[o.lane][1] += 1
        o.lane_seq = self.lanes[o.lane][1]
        o.sig = True
        o.idx = len(self.ops)
        o.fn = fn if fn is not None else (lambda e: e.dma_start(out=out, in_=in_, **kw))
        self._deps(o, reads, writes)
        self.ops.append(o)
        self.since.append(o)
        return o

    def barrier(self):
        if not hasattr(self, "_bar"):
            self._bar, self._bar_b = self.sb_root([128, 8], F32, "bar")
        prev = list(self.since)
        a = self._bar[:]
        o = self.op("pool", lambda e: e.memset(a, 0.0), writes=[self._bar_b])
        o.deps |= set(prev)
        o.deps.discard(o)
        self.epoch = o
        self.since = []
        return o

    def sb_root(self, shape, dtype, name):
        self.n_tiles += 1
        t = self.stack.enter_context(self.nc.sbuf_tensor(f"{name}_{self.n_tiles}", list(shape), dtype))
        return t, Buf(name)

    def mm(self, out, lhsT, rhs, start, stop, reads, writes):
        return self.op("pe", lambda e: e.matmul(out, lhsT=lhsT, rhs=rhs, start=start, stop=stop), reads, writes)

    def tr(self, out, in_, ident, reads, writes):
        return self.op("pe", lambda e: e.transpose(out=out, in_=in_, identity=ident), reads, writes)

    def act(self, out, in_, func, reads, writes, **kw):
        return self.op("act", lambda e: e.activation(out=out, in_=in_, func=func, **kw), reads, writes)

    def tt(self, eng, out, in0, in1, op, reads, writes):
        return self.op(eng, lambda e: e.tensor_tensor(out=out, in0=in0, in1=in1, op=op), reads, writes)

    def ts(self, eng, out, in0, s1, s2, op0, op1, reads, writes, **kw):
        if op1 is None:
            return self.op(eng, lambda e: e.tensor_scalar(out=out, in0=in0, scalar1=s1, scalar2=None, op0=op0, **kw), reads, writes)
        return self.op(eng, lambda e: e.tensor_scalar(out=out, in0=in0, scalar1=s1, scalar2=s2, op0=op0, op1=op1, **kw), reads, writes)

    def stt(self, eng, out, in0, scalar, in1, op0, op1, reads, writes):
        return self.op(eng, lambda e: e.scalar_tensor_tensor(out=out, in0=in0, scalar=scalar, in1=in1, op0=op0, op1=op1), reads, writes)

    def cp(self, eng, out, in_, reads, writes):
        if eng == "act":
            return self.op("act", lambda e: e.copy(out=out, in_=in_), reads, writes)
        return self.op(eng, lambda e: e.tensor_copy(out=out, in_=in_), reads, writes)

    def memset(self, eng, out, val, writes):
        return self.op(eng, lambda e: e.memset(out, val), (), writes)

    def emit(self, final_wait_ops=()):
        nc = self.nc
        ops = self.ops
        for o in ops:
            for d in o.deps:
                if not d.is_dma:
                    if d.eng == "pe" and o.eng == "pe" and not o.is_dma:
                        continue
                    d.sig = True
        for d in final_wait_ops:
            d.sig = True
        counts = {e: 0 for e in self.ENGS}
        for o in ops:
            if not o.is_dma and o.sig:
                counts[o.eng] += 1
                o.sig_idx = counts[o.eng]
        esems = {}
        for e in self.ENGS:
            n_ep = counts[e] // SEM_EPOCH + 1
            esems[e] = [self.stack.enter_context(nc.semaphore(f"s_{e}{k}")) for k in range(n_ep)]
        for k, l in enumerate(self.lanes):
            l[0] = self.stack.enter_context(nc.semaphore(f"l{k}"))
        self.n_sems = sum(len(v) for v in esems.values()) + len(self.lanes)

        def target(d):
            if d.is_dma:
                return (("l", d.lane), self.lanes[d.lane][0], 16 * d.lane_seq)
            ep, v = divmod(d.sig_idx - 1, SEM_EPOCH)
            return ((d.eng, ep), esems[d.eng][ep], v + 1)

        per_eng = {e: [o for o in ops if o.eng == e] for e in self.ENGS}
        block = self.stack.enter_context(nc.Block())

        def run(engname, e):
            waited = {}
            if engname == "pool":
                self.pool_reg = e.to_reg(NEXP * CAP - 1)
            for o in per_eng[engname]:
                need = {}
                for d in o.deps:
                    if (not d.is_dma) and (not o.is_dma) and d.eng == "pe" and o.eng == "pe":
                        continue
                    key, s, v = target(d)
                    if v > need.get(key, (None, 0))[1]:
                        need[key] = (s, v)
                for key, (s, v) in need.items():
                    if waited.get(key, 0) >= v:
                        continue
                    e.wait_ge(s, v)
                    waited[key] = v
                ins = o.fn(e)
                if o.is_dma:
                    ins.then_inc(self.lanes[o.lane][0], 16)
                elif o.sig:
                    ep = (o.sig_idx - 1) // SEM_EPOCH
                    ins.then_inc(esems[o.eng][ep], 1)
            if engname == "sp":
                for d in final_wait_ops:
                    key, s, v = target(d)
                    e.wait_ge(s, v)

        @block.tensor
        def _(e):
            run("pe", e)

        @block.scalar
        def _(e):
            run("act", e)

        @block.vector
        def _(e):
            run("dve", e)

        @block.gpsimd
        def _(e):
            run("pool", e)

        @block.sync
        def _(e):
            run("sp", e)


class RR:
    def __init__(self, items):
        self.items = items
        self.i = 0

    def next(self):
        it = self.items[self.i % len(self.items)]
        self.i += 1
        return it


def build(debug=(), stages=99):
    nc = bass.Bass("TRN2", target_bir_lowering=False)
    stack = ExitStack()
    P = Prog(nc, stack, debug)
    inp = {}

    def ext_in(name, shape, dtype=F32):
        inp[name] = nc.dram_tensor(name, list(shape), dtype, kind="ExternalInput").ap()
        return inp[name]

    xcat = ext_in("xcat", [TA, D])
    cvec = ext_in("cvec", [128, 8, 2])
    w_mod = ext_in("w_mod", [D, 6 * D])
    rowv = ext_in("rowv", [2, 8 * D])
    colv = ext_in("colv", [128, 8, 9])
    qkn = ext_in("qkn", [128, 5])
    w_in = ext_in("w_in", [D, 4864])
    w_uq = ext_in("w_uq", [384, 2048])
    w_ukv = ext_in("w_ukv", [256, 2048])
    ropet = ext_in("ropet", [2, 128, TA])
    cst = ext_in("cst", [128, 8, 128])
    w_bd = ext_in("w_bd", [128, 24, 128])
    w_gate = ext_in("w_gate", [128, 24, 16])
    b_gate = ext_in("b_gate", [16, 1])
    w_out = ext_in("w_out", [D, D])
    w_router = ext_in("w_router", [128, 8, 16])
    w_eg = ext_in("w_eg", [NEXP, D, D])
    w_eu = ext_in("w_eu", [NEXP, D, D])
    w_ed = ext_in("w_ed", [NEXP, D, D])
    tokid = ext_in("tokid", [128, 64], I32)
    out = nc.dram_tensor("out", [T, D], F32, kind="ExternalOutput").ap()

    XOFF_C, XOFF_L = 2, 262
    XMT, XMT_b = P.dram("XMT", [D, 8456], F32)
    ZS, ZS_b = P.dram("ZS", [D, T], BF16)
    GA, GA_b = P.dram("GA", [D, T], BF16)
    GM, GM_b = P.dram("GM", [D, T], BF16)
    QN, QN_b = P.dram("QN", [D, T], BF16)
    QR, QR_b = P.dram("QR", [512, T], BF16)
    KN, KN_b = P.dram("KN", [D, TA], BF16)
    KR, KR_b = P.dram("KR", [64, TA], BF16)
    VV, VV_b = P.dram("VV", [TA, D], BF16)
    XCT, XCT_b = P.dram("XCT", [D, T], F32)
    MQT, MQT_b = P.dram("MQT", [D, T], BF16)
    MKT, MKT_b = P.dram("MKT", [D, TA], BF16)
    MK, MK_b = P.dram("MK", [TA, D], BF16)
    MV, MV_b = P.dram("MV", [TA, D], BF16)
    OT, OT_b = P.dram("OT", [D, T], F32)
    HF, HF_b = P.dram("HF", [T, D], F32)
    HNT, HNT_b = P.dram("HNT", [D, T], F32)
    X1, X1_b = P.dram("X1", [T, D], F32)
    H2, H2_b = P.dram("H2", [T, D], BF16)
    XS, XS_b = P.dram("XS", [NEXP * CAP, D], BF16)
    YY, YY_b = P.dram("YY", [NEXP * CAP, D], F32)

    finals = []
    with stack:
        P._bar, P._bar_b = P.sb_root([128, 8], F32, "bar")
        cs, cs_b = P.sb([128, 8, 128], F32, "cst")
        csb, csb_b = P.sb([128, 8, 128], BF16, "cstb")
        colt, colt_b = P.sb([128, 8, 9], F32, "colv")
        qknt, qknt_b = P.sb([128, 5], F32, "qkn")
        modc, modc_b = P.sb([128, 48, 2], F32, "modc")
        s1c, s1c_b = P.sb([128, 8, 2], F32, "s1c")
        bc, bc_b = P.sb([128, 5, D], F32, "bc")
        gtm, gtm_b = P.sb([128, 66, 16], F32, "gtm")
        aff, aff_b = P.sb([128, 64, 16], F32, "aff")
        posi, posi_b = P.sb([128, 64 * 16], I32, "posi")
        gmv, gmv_b = P.sb([128, 64, 16], F32, "gmv")
        psb = [P.ps([128, 512], F32, f"ps{i}") for i in range(7)]
        psbf, psbf_b = P.ps([128, 1024], BF16, "psbf")
        P.dma("sp", cs[:], cst, writes=[cs_b], lane_buf=cs_b)
        P.dma("sp", colt[:], colv, writes=[colt_b], lane_buf=colt_b)
        P.dma("sp", qknt[:], qkn, writes=[qknt_b], lane_buf=qknt_b)
        P.cp("dve", csb[:], cs[:], [cs_b], [csb_b])
        ident = cs[:, 0, :]
        onesb = csb[:, 7, :]
        psr = RR(psb)

        with P.scope():
            cv, cv_b = P.sb([128, 8, 2], F32, "cv")
            sv, sv_b = P.sb([128, 8, 2], F32, "sv")
            rv, rv_b = P.sb([2, 8 * D], F32, "rv")
            mrow, mrow_b = P.sb([2, 8 * D], F32, "mrow")
            wms = [P.sb([128, 8, 512], F32, f"wm{i}") for i in range(2)]
            P.dma("sp", cv[:], cvec, writes=[cv_b], lane_buf=cv_b)
            P.dma("sp", rv[:], rowv, writes=[rv_b], lane_buf=rv_b)
            P.act(sv[:], cv[:], AF.Silu, [cv_b], [sv_b])
            for j in range(12):
                wm, wm_b = wms[j % 2]
                P.dma("sp" if j % 2 == 0 else "pool", wm[:], w_mod[:, j * 512:(j + 1) * 512].rearrange("(c p) n -> p c n", p=128),
                      writes=[wm_b], lane_buf=wm_b)
                pt, pt_b = psr.next()
                for k in range(8):
                    P.mm(pt[0:2, :], sv[:, k, :], wm[:, k, :], k == 0, k == 7, [sv_b, wm_b], [pt_b])
                P.tt("dve", mrow[:, j * 512:(j + 1) * 512], pt[0:2, :], rv[:, j * 512:(j + 1) * 512], ALU.add, [pt_b, rv_b], [mrow_b])
            P.cp("dve", mrow[:, 6 * D:8 * D], rv[:, 6 * D:8 * D], [rv_b], [mrow_b])
            for g in range(6):
                pt, pt_b = psr.next()
                for c in range(8):
                    j = g * 8 + c
                    P.tr(pt[:, c * 2:c * 2 + 2], mrow[:, j * 128:(j + 1) * 128], cs[0:2, 0, 0:2], [mrow_b, cs_b], [pt_b])
                P.cp("dve", modc[:, g * 8:(g + 1) * 8, :].rearrange("p c v -> p (c v)"), pt[:, 0:16], [pt_b], [modc_b])
            for v in range(2):
                P.stt("dve", s1c[:, :, v], modc[:, 8:16, v], 1.0, colt[:, :, 0], ALU.add, ALU.mult, [modc_b, colt_b], [s1c_b])
            srcs = [2 * D, 4 * D, 3 * D, 5 * D, 7 * D, 6 * D]
            n2b, n2b_b = P.sb([128, D], F32, "n2b")
            for i, off in enumerate(srcs):
                for hh in range(2):
                    pt, pt_b = psr.next()
                    P.mm(pt[:, :], cs[0:2, 6, :], mrow[:, off + hh * 512: off + (hh + 1) * 512], True, True, [cs_b, mrow_b], [pt_b])
                    if i < 5:
                        P.cp("dve", bc[:, i, hh * 512:(hh + 1) * 512], pt[:, :], [pt_b], [bc_b])
                    else:
                        P.cp("dve", n2b[:, hh * 512:(hh + 1) * 512], pt[:, :], [pt_b], [n2b_b])
            P.stt("dve", bc[:, 1, :], bc[:, 1, :], 1.0, n2b[:], ALU.add, ALU.mult, [bc_b, n2b_b], [bc_b])
            if "DBG_mod" in P.debug:
                dm, dm_b = P.dram("DBG_mod", [128, 96], F32)
                finals.append(P.dma("sp", dm, modc[:].rearrange("p c v -> p (c v)"), reads=[modc_b], lane_buf=modc_b))
                db, db_b = P.dram("DBG_bc", [128, 5 * D], F32)
                finals.append(P.dma("sp", db, bc[:].rearrange("p c v -> p (c v)"), reads=[bc_b], lane_buf=bc_b))

        def stage1(pass_b):
          with P.scope():
            ncol = 3072 if pass_b else 1792
            win, win_b = P.sb([128, 8, ncol], BF16, "win")
            if not pass_b:
                wuq, wuq_b = P.sb([128, 3, 2048], BF16, "wuq")
                wukv, wukv_b = P.sb([128, 2, 2048], BF16, "wukv")
            xin = RR([P.sb([128, 4, D], F32, f"xin{i}") for i in range(2)])
            hT = RR([P.sb([128, 8, 512], BF16, f"hT{i}") for i in range(2)])
            sqj, sqj_b = P.sb([128, D], BF16, "sqj")
            ssr = RR([P.sb([128, 12], F32, f"ss{i}") for i in range(2)])
            dgr = RR([P.sb([128, 4, 128], F32, f"dg{i}") for i in range(2)])
            if not pass_b:
              qlf, qlf_b = P.sb([128, 5, 512], F32, "qlf")
              sqb, sqb_b = P.sb([128, 5, 512], BF16, "sqb")
              rq, rq_b = P.sb([128, 2, 512], F32, "rq")
              qn, qn_b = P.sb([128, 5, 512], BF16, "qn")
              rope, rope_b = P.sb([128, 2, 512], F32, "rope")
              rt1, rt1_b = P.sb([128, 512], F32, "rt1")
              rt2, rt2_b = P.sb([128, 512], F32, "rt2")
              ob16 = RR([P.sb([128, 512], BF16, f"ob{i}") for i in range(4)])
              vb16 = RR([P.sb([128, D], BF16, f"vb{i}") for i in range(2)])
              zpad, zpad_b = P.sb([128, 8, 2], F32, "zpad")
            of32 = RR([P.sb([128, 512], F32, f"of{i}") for i in range(4)])
            of16 = RR([P.sb([128, 512], BF16, f"of16{i}") for i in range(4)])
            dq = RR(["sp", "pool"] if "USEPOOL" in P.debug else ["sp"])

            srccols = [1728 + j * 256 for j in range(12)] if pass_b else ([j * 256 for j in range(6)] + [1536, 4800])
            dstcol = 0
            for j, sc_ in enumerate(srccols):
                w = 256
                if not pass_b and j == 6:
                    w = 192
                if not pass_b and j == 7:
                    w = 64
                P.dma("pool", win[:, :, dstcol:dstcol + w], w_in[:, sc_:sc_ + w].rearrange("(c p) n -> p c n", p=128), writes=[win_b], lane_buf=win_b)
                dstcol += w
            if not pass_b:
                P.memset("dve", zpad[:], 0.0, [zpad_b])
                for off in (0, 258, 260, 260 + 2 + T):
                    P.dma("sp", XMT[:, off:off + 2].rearrange("(c p) n -> p c n", p=128), zpad[:], reads=[zpad_b], writes=[XMT_b], lane_buf=zpad_b)
                for j in range(4):
                    P.dma("pool", wuq[:, :, j * 512:(j + 1) * 512], w_uq[:, j * 512:(j + 1) * 512].rearrange("(c p) n -> p c n", p=128), writes=[wuq_b], lane_buf=wuq_b)
                for j in range(4):
                    P.dma("pool", wukv[:, :, j * 512:(j + 1) * 512], w_ukv[:, j * 512:(j + 1) * 512].rearrange("(c p) n -> p c n", p=128), writes=[wukv_b], lane_buf=wukv_b)

            blocks = [(0, 256, True)] + [(TC + i * 512, 512, False) for i in range(T // 512)]
            LIM = int(os.environ.get("S1LIM", "99"))
            blocks = blocks[:int(os.environ.get("S1NBLK", "99"))]
            if LIM < 1:
                blocks = []
            def block_gen(t0, NT, is_ctx):
                nt = NT // 128
                var = 1 if is_ctx else 0
                tq = t0 - TC
                xoff = (XOFF_C + t0) if is_ctx else (XOFF_L + tq)
                xi, xi_b = xin.next()
                P.dma(dq.next(), xi[:, 0:nt, :], xcat[t0:t0 + NT, :].rearrange("(i p) f -> p i f", p=128), writes=[xi_b], lane_buf=xi_b)
                ss, ss_b = ssr.next()
                dg, dg_b = dgr.next()
                P.memset("dve", ss[:], 0.0, [ss_b])
                for i in range(nt):
                    P.act(sqj[:], xi[:, i, :], AF.Square, [xi_b, ss_b], [sqj_b, ss_b], accum_out=ss[:, i:i + 1])
                P.act(ss[:, 4:4 + nt], ss[:, 0:nt], AF.Ln, [ss_b], [ss_b], scale=1.0 / D, bias=EPS)
                P.act(ss[:, 8:8 + nt], ss[:, 4:4 + nt], AF.Exp, [ss_b], [ss_b], scale=-0.5)
                for i in range(nt):
                    P.ts("dve", dg[:, i, :], ident, ss[:, 8 + i:9 + i], None, ALU.mult, None, [cs_b, ss_b], [dg_b])
                h, h_b = hT.next()
                for c in range(8):
                    pt, pt_b = psr.next()
                    for i in range(nt):
                        P.mm(pt[:, i * 128:(i + 1) * 128], xi[:, i, c * 128:(c + 1) * 128], dg[:, i, :], True, True, [xi_b, dg_b], [pt_b])
                    P.act(h[:, c, 0:NT], pt[:, 0:NT], AF.Identity, [pt_b, s1c_b, modc_b], [h_b],
                          scale=s1c[:, c, var:var + 1], bias=modc[:, c, var:var + 1])

                yield
                if not pass_b:
                    P.dma(dq.next(), rope[:, :, 0:NT], ropet[:, :, t0:t0 + NT].rearrange("a p t -> p a t"), writes=[rope_b], lane_buf=rope_b)
                def gemm(col0, ncols, pt, pt_b):
                    for k in range(8):
                        P.mm(pt[0:ncols, 0:NT], win[:, k, col0:col0 + ncols], h[:, k, 0:NT], k == 0, k == 7, [win_b, h_b], [pt_b])

                if pass_b:
                    for gi, (dst, dst_b) in enumerate(((ZS, ZS_b), (GA, GA_b), (GM, GM_b))):
                        for c in range(8):
                            pt, pt_b = psr.next()
                            gemm(gi * 1024 + c * 128, 128, pt, pt_b)
                            of, of_b = of16.next()
                            P.act(of[:, 0:NT], pt[:, 0:NT], AF.Sigmoid, [pt_b], [of_b])
                            P.dma(dq.next(), dst[c * 128:(c + 1) * 128, tq:tq + NT], of[:, 0:NT], reads=[of_b], writes=[dst_b], lane_buf=of_b)
                    return
                if LIM < 2:
                    return
                lat = range(0, 5) if not is_ctx else range(3, 5)
                for c in lat:
                    pt, pt_b = psr.next()
                    gemm(c * 128, 128, pt, pt_b)
                    P.cp("dve", qlf[:, c, 0:NT], pt[:, 0:NT], [pt_b], [qlf_b])
                    P.act(sqb[:, c, 0:NT], pt[:, 0:NT], AF.Square, [pt_b], [sqb_b])
                groups = ([(0, 0, 3, 384.0)] if not is_ctx else []) + [(1, 3, 2, 256.0)]
                for (gi, c0, ncx, nfeat) in groups:
                    pt, pt_b = psr.next()
                    for k in range(ncx):
                        P.mm(pt[:, 0:NT], onesb, sqb[:, c0 + k, 0:NT], k == 0, k == ncx - 1, [csb_b, sqb_b], [pt_b])
                    P.act(rq[:, gi, 0:NT], pt[:, 0:NT], AF.Ln, [pt_b], [rq_b], scale=1.0 / nfeat, bias=EPS)
                    P.act(rq[:, gi, 0:NT], rq[:, gi, 0:NT], AF.Exp, [rq_b], [rq_b], scale=-0.5)
                    for k in range(ncx):
                        c = c0 + k
                        P.stt("dve", qn[:, c, 0:NT], qlf[:, c, 0:NT], qknt[:, c:c + 1], rq[:, gi, 0:NT], ALU.mult, ALU.mult,
                              [qlf_b, qknt_b, rq_b], [qn_b])
                if LIM < 3:
                    return
                if not is_ctx:
                    for hd in range(8):
                        pt, pt_b = psr.next()
                        for k in range(3):
                            P.mm(pt[:, 0:NT], wuq[:, k, hd * 128:(hd + 1) * 128], qn[:, k, 0:NT], k == 0, k == 2, [wuq_b, qn_b], [pt_b])
                        ob, ob_b = ob16.next()
                        P.cp("act", ob[:, 0:NT], pt[:, 0:NT], [pt_b], [ob_b])
                        P.dma(dq.next(), QN[hd * 128:(hd + 1) * 128, tq:tq + NT], ob[:, 0:NT], reads=[ob_b], writes=[QN_b], lane_buf=ob_b)
                    for hp in range(4):
                        p1, p1_b = psr.next()
                        p2, p2_b = psr.next()
                        for k in range(3):
                            P.mm(p1[:, 0:NT], wuq[:, k, 1024 + hp * 128:1024 + (hp + 1) * 128], qn[:, k, 0:NT], k == 0, k == 2, [wuq_b, qn_b], [p1_b])
                        for k in range(3):
                            P.mm(p2[:, 0:NT], wuq[:, k, 1536 + hp * 128:1536 + (hp + 1) * 128], qn[:, k, 0:NT], k == 0, k == 2, [wuq_b, qn_b], [p2_b])
                        P.tt("dve", rt1[:, 0:NT], p1[:, 0:NT], rope[:, 0, 0:NT], ALU.mult, [p1_b, rope_b], [rt1_b])
                        P.tt("dve", rt2[:, 0:NT], p2[:, 0:NT], rope[:, 1, 0:NT], ALU.mult, [p2_b, rope_b], [rt2_b])
                        ob, ob_b = ob16.next()
                        P.tt("pool", ob[:, 0:NT], rt1[:, 0:NT], rt2[:, 0:NT], ALU.add, [rt1_b, rt2_b], [ob_b])
                        P.dma(dq.next(), QR[hp * 128:(hp + 1) * 128, tq:tq + NT], ob[:, 0:NT], reads=[ob_b], writes=[QR_b], lane_buf=ob_b)
                if LIM < 4:
                    return
                for hd in range(8):
                    pt, pt_b = psr.next()
                    for k in range(2):
                        P.mm(pt[:, 0:NT], wukv[:, k, hd * 128:(hd + 1) * 128], qn[:, 3 + k, 0:NT], k == 0, k == 1, [wukv_b, qn_b], [pt_b])
                    ob, ob_b = ob16.next()
                    P.cp("act", ob[:, 0:NT], pt[:, 0:NT], [pt_b], [ob_b])
                    P.dma(dq.next(), KN[hd * 128:(hd + 1) * 128, t0:t0 + NT], ob[:, 0:NT], reads=[ob_b], writes=[KN_b], lane_buf=ob_b)
                if LIM < 5:
                    return
                for i in range(nt):
                    vb, vb_b = vb16.next()
                    for hh in range(2):
                        pt, pt_b = psr.next()
                        for k in range(2):
                            P.mm(pt[:, :], qn[:, 3 + k, i * 128:(i + 1) * 128], wukv[:, k, 1024 + hh * 512:1024 + (hh + 1) * 512], k == 0, k == 1,
                                 [qn_b, wukv_b], [pt_b])
                        P.cp("act" if hh else "dve", vb[:, hh * 512:(hh + 1) * 512], pt[:, :], [pt_b], [vb_b])
                    P.dma(dq.next(), VV[t0 + i * 128:t0 + (i + 1) * 128, :], vb[:], reads=[vb_b], writes=[VV_b], lane_buf=vb_b)
                if LIM < 6:
                    return
                p1, p1_b = psr.next()
                p2, p2_b = psr.next()
                gemm(640, 64, p1, p1_b)
                gemm(1728, 64, p2, p2_b)
                P.tt("dve", rt1[0:64, 0:NT], p1[0:64, 0:NT], rope[0:64, 0, 0:NT], ALU.mult, [p1_b, rope_b], [rt1_b])
                P.tt("dve", rt2[0:64, 0:NT], p2[0:64, 0:NT], rope[0:64, 1, 0:NT], ALU.mult, [p2_b, rope_b], [rt2_b])
                ob, ob_b = ob16.next()
                P.tt("pool", ob[0:64, 0:NT], rt1[0:64, 0:NT], rt2[0:64, 0:NT], ALU.add, [rt1_b, rt2_b], [ob_b])
                P.dma(dq.next(), KR[:, t0:t0 + NT], ob[0:64, 0:NT], reads=[ob_b], writes=[KR_b], lane_buf=ob_b)
                if LIM < 7:
                    return
                for c in range(8):
                    pt, pt_b = psr.next()
                    gemm(704 + c * 128, 128, pt, pt_b)
                    of, of_b = of32.next()
                    P.cp("act" if c % 2 else "dve", of[:, 0:NT], pt[:, 0:NT], [pt_b], [of_b])
                    P.dma(dq.next(), XMT[c * 128:(c + 1) * 128, xoff:xoff + NT], of[:, 0:NT], reads=[of_b], writes=[XMT_b], lane_buf=of_b)

            todo = [b for b in blocks if not (pass_b and b[2])]
            gens = [block_gen(*b) for b in todo]
            if gens:
                next(gens[0])
            for gi_ in range(len(gens)):
                if gi_ + 1 < len(gens):
                    next(gens[gi_ + 1])
                for _ in gens[gi_]:
                    pass

        ST = set(os.environ.get("STAGES", "1a,1b,2,attn,scan,merge,moe,final").split(","))
        if "1a" in ST:
            stage1(False)
        if "1b" in ST:
            stage1(True)


        def stage2():
          with P.scope():
            wst, wst_b = P.sb([128, 24, 128], F32, "wst")
            wbd16, wbd16_b = P.sb([128, 24, 128], BF16, "wbd16")
            wgs, wgs_b = P.sb([128, 24, 16], F32, "wgs")
            wg16, wg16_b = P.sb([128, 24, 16], BF16, "wg16")
            bg, bg_b = P.sb([16, 1], F32, "bg")
            xm32 = RR([P.sb([128, 516], F32, f"xm32{i}") for i in range(4)])
            accr = RR([P.sb([128, 512], F32, f"acc{i}") for i in range(2)])
            xcfr = RR([P.sb([128, 512], F32, f"xcf{i}") for i in range(2)])
            xcbr = RR([P.sb([128, 512], BF16, f"xcb{i}") for i in range(2)])
            xmbr = RR([P.sb([128, 512], BF16, f"xmb{i}") for i in range(2)])
            qtbr = RR([P.sb([128, 512], BF16, f"qtb{i}") for i in range(2)])
            ktsr = RR([P.sb([128, 512], BF16, f"kts{i}") for i in range(2)])
            vtbr = RR([P.sb([128, 512], BF16, f"vtb{i}") for i in range(2)])
            ktmr = RR([P.sb([128, 512], BF16, f"ktm{i}") for i in range(2)])
            vtmr = RR([P.sb([128, 512], BF16, f"vtm{i}") for i in range(2)])
            gts, gts_b = P.sb([16, 512], F32, "gts")
            lft, lft_b = P.sb([128, 66, 4], F32, "lft")
            dq = RR(["sp"])
            psr2 = RR(psb[0:6])
            pg, pg_b = psb[6]
            P.memset("dve", gtm[:], 0.0, [gtm_b])
            P.dma("sp", wst[:], w_bd, writes=[wst_b], lane_buf=wst_b)
            P.cp("dve", wbd16[:], wst[:], [wst_b], [wbd16_b])
            P.dma("sp", wgs[:], w_gate, writes=[wgs_b], lane_buf=wgs_b)
            P.dma("sp", bg[:], b_gate, writes=[bg_b], lane_buf=bg_b)
            P.cp("dve", wg16[:, 0:8, :], wgs[:, 0:8, :], [wgs_b], [wg16_b])
            P.ts("dve", wg16[:, 8:16, :], wgs[:, 8:16, :], 16.0, None, ALU.mult, None, [wgs_b], [wg16_b])
            P.cp("dve", wg16[:, 16:24, :], wgs[:, 16:24, :], [wgs_b], [wg16_b])
            blocks = [(0, 256, True)] + [(TC + i * 512, 512, False) for i in range(T // 512)]
            blocks = blocks[:int(os.environ.get("S2NBLK", "99"))]

            def chunk_gen(t0, NT, is_ctx, c):
                nt = NT // 128
                tq = t0 - TC
                xoff = (XOFF_C + t0) if is_ctx else (XOFF_L + tq)
                xm, xm_b = xm32.next()
                P.dma(dq.next(), xm[:, 0:NT + 4], XMT[c * 128:(c + 1) * 128, xoff - 2:xoff + NT + 2], reads=[XMT_b], writes=[xm_b], lane_buf=xm_b)
                acc, acc_b = accr.next()
                P.ts("dve", acc[:, 0:NT], xm[:, 0:NT], colt[:, c, 4:5], colt[:, c, 1:2], ALU.mult, ALU.add, [xm_b, colt_b], [acc_b])
                for j in range(1, 5):
                    P.stt("dve", acc[:, 0:NT], xm[:, j:j + NT], colt[:, c, 4 + j:5 + j], acc[:, 0:NT], ALU.mult, ALU.add, [xm_b, colt_b, acc_b], [acc_b])
                xcf, xcf_b = xcfr.next()
                P.act(xcf[:, 0:NT], acc[:, 0:NT], AF.Silu, [acc_b], [xcf_b])
                if not is_ctx:
                    P.dma(dq.next(), XCT[c * 128:(c + 1) * 128, tq:tq + NT], xcf[:, 0:NT], reads=[xcf_b], writes=[XCT_b], lane_buf=xcf_b)
                xcb, xcb_b = xcbr.next()
                xmb, xmb_b = xmbr.next()
                P.cp("act", xcb[:, 0:NT], xcf[:, 0:NT], [xcf_b], [xcb_b])
                P.cp("act", xmb[:, 0:NT], xm[:, 2:2 + NT], [xm_b], [xmb_b])
                yield
                qtb, qtb_b = qtbr.next()
                kts, kts_b = ktsr.next()
                vtb, vtb_b = vtbr.next()
                pt, pt_b = psr2.next()
                P.mm(pt[:, 0:NT], wbd16[:, c, :], xcb[:, 0:NT], True, True, [wbd16_b, xcb_b], [pt_b])
                P.cp("act", qtb[:, 0:NT], pt[:, 0:NT], [pt_b], [qtb_b])
                if not is_ctx:
                    P.dma(dq.next(), MQT[c * 128:(c + 1) * 128, tq:tq + NT], qtb[:, 0:NT], reads=[qtb_b], writes=[MQT_b], lane_buf=qtb_b)
                pt, pt_b = psr2.next()
                P.mm(pt[:, 0:NT], wbd16[:, 8 + c, :], xcb[:, 0:NT], True, True, [wbd16_b, xcb_b], [pt_b])
                P.act(kts[:, 0:NT], pt[:, 0:NT], AF.Copy, [pt_b], [kts_b], scale=1.0 / 16.0)
                P.dma(dq.next(), MKT[c * 128:(c + 1) * 128, t0:t0 + NT], kts[:, 0:NT], reads=[kts_b], writes=[MKT_b], lane_buf=kts_b)
                pt, pt_b = psr2.next()
                P.mm(pt[:, 0:NT], wbd16[:, 16 + c, :], xmb[:, 0:NT], True, True, [wbd16_b, xmb_b], [pt_b])
                P.cp("dve", vtb[:, 0:NT], pt[:, 0:NT], [pt_b], [vtb_b])
                P.mm(pg[0:16, 0:NT], wg16[:, c, :], qtb[:, 0:NT], c == 0, False, [wg16_b, qtb_b], [pg_b])
                P.mm(pg[0:16, 0:NT], wg16[:, 8 + c, :], kts[:, 0:NT], False, False, [wg16_b, kts_b], [pg_b])
                P.mm(pg[0:16, 0:NT], wg16[:, 16 + c, :], vtb[:, 0:NT], False, c == 7, [wg16_b, vtb_b], [pg_b])
                ktm, ktm_b = ktmr.next()
                vtm, vtm_b = vtmr.next()
                pt, pt_b = psr2.next()
                for i in range(nt):
                    P.mm(pt[:, i * 128:(i + 1) * 128], xcb[:, i * 128:(i + 1) * 128], wbd16[:, 8 + c, :], True, True, [xcb_b, wbd16_b], [pt_b])
                P.act(ktm[:, 0:NT], pt[:, 0:NT], AF.Copy, [pt_b], [ktm_b], scale=1.0 / 16.0)
                P.dma(dq.next(), MK[t0:t0 + NT, c * 128:(c + 1) * 128].rearrange("(i p) d -> p i d", p=128),
                      ktm[:, 0:NT].rearrange("p (i d) -> p i d", d=128), reads=[ktm_b], writes=[MK_b], lane_buf=ktm_b)
                pt, pt_b = psr2.next()
                for i in range(nt):
                    P.mm(pt[:, i * 128:(i + 1) * 128], xmb[:, i * 128:(i + 1) * 128], wbd16[:, 16 + c, :], True, True, [xmb_b, wbd16_b], [pt_b])
                P.cp("dve", vtm[:, 0:NT], pt[:, 0:NT], [pt_b], [vtm_b])
                P.dma(dq.next(), MV[t0:t0 + NT, c * 128:(c + 1) * 128].rearrange("(i p) d -> p i d", p=128),
                      vtm[:, 0:NT].rearrange("p (i d) -> p i d", d=128), reads=[vtm_b], writes=[MV_b], lane_buf=vtm_b)
                if c == 7:
                    P.act(gts[:, 0:NT], pg[0:16, 0:NT], AF.Identity, [pg_b, bg_b], [gts_b], bias=bg[:, 0:1])
                    pt, pt_b = psr2.next()
                    for i in range(nt):
                        P.tr(pt[:, i * 16:(i + 1) * 16], gts[:, i * 128:(i + 1) * 128], cs[0:16, 0, 0:16], [gts_b, cs_b], [pt_b])
                    j0 = t0 // 128
                    P.cp("dve", gtm[:, j0:j0 + nt, :], pt[:, 0:nt * 16].rearrange("p (i g) -> p i g", g=16), [pt_b], [gtm_b])

            gens = [chunk_gen(t0, NT, is_ctx, c) for (t0, NT, is_ctx) in blocks for c in range(8)]
            if gens:
                next(gens[0])
            for gi_ in range(len(gens)):
                if gi_ + 1 < len(gens):
                    next(gens[gi_ + 1])
                for _ in gens[gi_]:
                    pass
            for d_ in range(2):
                fv = gtm[:, :, d_ * 8 + 4:d_ * 8 + 8]
                P.act(lft[:], fv, AF.Exp, [gtm_b], [lft_b], scale=-1.0)
                P.act(lft[:], lft[:], AF.Ln, [lft_b], [lft_b], bias=1.0)
                P.ts("dve", fv, lft[:], -1.0, None, ALU.mult, None, [lft_b], [gtm_b])
            if "DBG_gtm" in P.debug:
                dg_, dg_b_ = P.dram("DBG_gtm", [128, 66 * 16], F32)
                finals.append(P.dma("sp", dg_, gtm[:].rearrange("p j g -> p (j g)"), reads=[gtm_b], lane_buf=gtm_b))

        def stage_attn():
          with P.scope():
            KA, KA_b = P.sb([128, TA], BF16, "KA")
            KB, KB_b = P.sb([128, TA], BF16, "KB")
            Vt, Vt_b = P.sb([128, 66, 128], BF16, "Vt")
            QA, QA_b = P.sb([128, T], BF16, "QA")
            QB, QB_b = P.sb([128, T], BF16, "QB")
            pTr = RR([P.sb([128, 512], BF16, f"pT{i}") for i in range(6)])
            s2r = RR([P.sb([128, 512], BF16, f"ps2{i}") for i in range(4)])
            s4r = RR([P.sb([128, 512], BF16, f"ps4{i}") for i in range(3)])
            rinvr = RR([P.sb([128, 512], F32, f"rinv{i}") for i in range(2)])
            osbr = RR([P.sb([128, 512], F32, f"osb{i}") for i in range(2)])
            Sr = RR(psb[0:4])
            Or = RR(psb[4:6])
            Mr = RR(psb[6:7])
            NH = int(os.environ.get("ATT_NH", "8"))
            NQG = int(os.environ.get("ATT_NQG", "16"))
            scale = float((128 + 64) ** -0.5)
            for c0 in range(0, TA, 2112):
                P.memset("dve", KB[64:128, c0:c0 + 2112], 0.0, [KB_b])
            for c0 in range(0, T, 2048):
                P.memset("dve", QB[64:128, c0:c0 + 2048], 0.0, [QB_b])
            P.dma("sp", KB[0:64, 0:4224], KR[:, 0:4224], reads=[KR_b], writes=[KB_b], lane_buf=KB_b)
            P.dma("sp", KB[0:64, 4224:TA], KR[:, 4224:TA], reads=[KR_b], writes=[KB_b], lane_buf=KB_b)
            for h in range(NH):
                P.dma("sp", KA[:, 0:4224], KN[h * 128:(h + 1) * 128, 0:4224], reads=[KN_b], writes=[KA_b], lane_buf=KA_b)
                P.dma("sp", KA[:, 4224:TA], KN[h * 128:(h + 1) * 128, 4224:TA], reads=[KN_b], writes=[KA_b], lane_buf=KA_b)
                P.dma("sp", QA[:, 0:4096], QN[h * 128:(h + 1) * 128, 0:4096], reads=[QN_b], writes=[QA_b], lane_buf=QA_b)
                P.dma("sp", QA[:, 4096:T], QN[h * 128:(h + 1) * 128, 4096:T], reads=[QN_b], writes=[QA_b], lane_buf=QA_b)
                P.dma("sp", QB[0:64, :], QR[h * 64:(h + 1) * 64, :], reads=[QR_b], writes=[QB_b], lane_buf=QB_b)
                P.dma("sp", Vt[:, 0:33, :], VV[0:4224, h * 128:(h + 1) * 128].rearrange("(j p) d -> p j d", p=128), reads=[VV_b], writes=[Vt_b], lane_buf=Vt_b)
                P.dma("sp", Vt[:, 33:66, :], VV[4224:TA, h * 128:(h + 1) * 128].rearrange("(j p) d -> p j d", p=128), reads=[VV_b], writes=[Vt_b], lane_buf=Vt_b)
                for qg in range(NQG):
                    q0 = qg * 512
                    O, O_b = Or.next()
                    M, M_b = Mr.next()

                    def qk(j):
                        S, S_b = Sr.next()
                        P.mm(S[:, :], KA[:, j * 128:(j + 1) * 128], QA[:, q0:q0 + 512], True, False, [KA_b, QA_b], [S_b])
                        P.mm(S[:, :], KB[:, j * 128:(j + 1) * 128], QB[:, q0:q0 + 512], False, True, [KB_b, QB_b], [S_b])
                        return S, S_b
                    sq_ = [qk(0), qk(1)]
                    prev_pT = None
                    hold = None
                    pend = []
                    nsum = 0
                    for j in range(66):
                        if j + 2 < 66:
                            sq_.append(qk(j + 2))
                        S, S_b = sq_.pop(0)
                        pT, pT_b = pTr.next()
                        P.act(pT[:, :], S[:, :], AF.Exp, [S_b], [pT_b], scale=scale)
                        P.mm(O[:, :], Vt[:, j, :], pT[:, :], j == 0, j == 65, [Vt_b, pT_b], [O_b])
                        if j % 2 == 1:
                            if pend and (j % 4 == 1):
                                s4, s4_b = pend.pop(0)
                                P.mm(M[:, :], onesb, s4[:, :], nsum == 0, False, [csb_b, s4_b], [M_b])
                                nsum += 1
                            s2, s2_b = s2r.next()
                            P.tt("dve", s2[:, :], prev_pT[0][:, :], pT[:, :], ALU.add, [prev_pT[1], pT_b], [s2_b])
                            if j % 4 == 3:
                                s4, s4_b = s4r.next()
                                P.tt("dve", s4[:, :], hold[0][:, :], s2[:, :], ALU.add, [hold[1], s2_b], [s4_b])
                                pend.append((s4, s4_b))
                            elif j == 65:
                                pend.append((s2, s2_b))
                            else:
                                hold = (s2, s2_b)
                        prev_pT = (pT, pT_b)
                    while pend:
                        s4, s4_b = pend.pop(0)
                        P.mm(M[:, :], onesb, s4[:, :], nsum == 0, len(pend) == 0, [csb_b, s4_b], [M_b])
                        nsum += 1
                    rinv, rinv_b = rinvr.next()
                    osb, osb_b = osbr.next()
                    P.op("dve", (lambda e, a=rinv[:, :], b=M[:, :]: e.reciprocal(out=a, in_=b)), [M_b], [rinv_b])
                    P.tt("dve", osb[:, :], O[:, :], rinv[:, :], ALU.mult, [O_b, rinv_b], [osb_b])
                    P.dma("sp", OT[h * 128:(h + 1) * 128, q0:q0 + 512], osb[:, :], reads=[osb_b], writes=[OT_b], lane_buf=osb_b)


        def stage_scan():
          with P.scope():
            CT, CT_b = P.sb([128, 2, 257], F32, "CT")
            CTb, CTb_b = P.sb([128, 2, 257], BF16, "CTb")
            kTg = RR([P.sb([128, 2, 1024], BF16, f"kTg{i}") for i in range(2)])
            qTg = RR([P.sb([128, 2, 1024], BF16, f"qTg{i}") for i in range(2)])
            ktmg = RR([P.sb([128, 8, 256], BF16, f"ktmg{i}") for i in range(2)])
            vtmg = RR([P.sb([128, 8, 257], BF16, f"vtmg{i}") for i in range(2)])
            LFr = RR([P.sb([128, 128], F32, f"LF{i}") for i in range(4)])
            Bsr = RR([P.sb([128, 129], F32, f"Bs{i}") for i in range(4)])
            rcr = RR([P.sb([128, 8], F32, f"rc{i}") for i in range(8)])
            DTr = RR([P.sb([128, 128], F32, f"DT{i}") for i in range(4)])
            ATr = RR([P.sb([128, 128], F32, f"AT{i}") for i in range(4)])
            Ebr = RR([P.sb([128, 128], F32, f"Eb{i}") for i in range(4)])
            vwr = RR([P.sb([128, 257], BF16, f"vw{i}") for i in range(4)])
            STr = RR([P.sb([128, 128], BF16, f"ST{i}") for i in range(4)])
            qsr = RR([P.sb([128, 2, 128], BF16, f"qs{i}") for i in range(4)])
            hchr = RR([P.sb([128, 256], F32, f"hch{i}") for i in range(4)])
            hflr = RR([P.sb([128, 256], F32, f"hfl{i}") for i in range(4)])
            hsr = RR([P.sb([128, 256], F32, f"hs{i}") for i in range(4)])
            hnr = RR([P.sb([128, 256], F32, f"hn{i}") for i in range(4)])
            hnTr = RR([P.sb([128, 2, 128], F32, f"hnT{i}") for i in range(4)])
            sqh, sqh_b = P.sb([128, 256], BF16, "sqh")
            for (vt, vt_b) in vtmg.items:
                P.memset("pool", vt[:], 1.0, [vt_b])
            dq = RR(["sp"])
            NHD = int(os.environ.get("SCAN_NH", "4"))
            NGRP = int(os.environ.get("SCAN_NG", "8"))
            onesf = cs[:, 7, :]
            groups = [(0, 2)] + [(2 + 8 * g, 8) for g in range(NGRP)]
            for hd in range(NHD):
                for d_ in range(2):
                    P.memset("dve", CT[:], 0.0, [CT_b])
                    P.memset("pool", CTb[:], 0.0, [CTb_b])
                    Umat = cs[:, 1, :] if d_ == 0 else cs[:, 2, :]
                    maskT = cs[:, 3, :] if d_ == 0 else cs[:, 4, :]
                    lc = 127 if d_ == 0 else 0
                    gi = d_ * 8 + hd
                    fi = d_ * 8 + 4 + hd
                    gorder = groups if d_ == 0 else [groups[0]] + groups[1:][::-1]
                    seq = []
                    for (j0, ng) in gorder:
                        jjs = list(range(ng)) if d_ == 0 else list(range(ng))[::-1]
                        for jj in jjs:
                            seq.append((j0, ng, jj))
                    gstate = {}

                    def load_group(j0, ng):
                        latent = j0 >= 2
                        t0 = j0 * 128
                        ntok = ng * 128
                        kt_, kt_b = ktmg.next()
                        vt_, vt_b = vtmg.next()
                        P.dma(dq.next(), kt_[:, 0:ng, :], MK[t0:t0 + ntok, hd * 256:(hd + 1) * 256].rearrange("(i p) d -> p i d", p=128),
                              reads=[MK_b], writes=[kt_b], lane_buf=kt_b)
                        P.dma(dq.next(), vt_[:, 0:ng, 0:256], MV[t0:t0 + ntok, hd * 256:(hd + 1) * 256].rearrange("(i p) d -> p i d", p=128),
                              reads=[MV_b], writes=[vt_b], lane_buf=vt_b)
                        g = dict(kt=(kt_, kt_b), vt=(vt_, vt_b))
                        if latent:
                            kT_, kT_b = kTg.next()
                            qT_, qT_b = qTg.next()
                            P.dma(dq.next(), kT_[:, :, 0:ntok], MKT[hd * 256:(hd + 1) * 256, t0:t0 + ntok].rearrange("(c p) t -> p c t", p=128),
                                  reads=[MKT_b], writes=[kT_b], lane_buf=kT_b)
                            P.dma(dq.next(), qT_[:, :, 0:ntok], MQT[hd * 256:(hd + 1) * 256, t0 - TC:t0 - TC + ntok].rearrange("(c p) t -> p c t", p=128),
                                  reads=[MQT_b], writes=[qT_b], lane_buf=qT_b)
                            g["kT"] = (kT_, kT_b)
                            g["qT"] = (qT_, qT_b)
                        return g

                    def get_group(j0, ng):
                        if j0 not in gstate:
                            gstate.clear()
                            gstate[j0] = load_group(j0, ng)
                        return gstate[j0]

                    def prepA(j0, ng, jj):
                        j = j0 + jj
                        lfc = gtm[:, j, fi:fi + 1]
                        LF, LF_b = LFr.next()
                        P.ts("dve", LF[:], onesf, lfc, None, ALU.mult, None, [cs_b, gtm_b], [LF_b])
                        pB, pB_b = psr.next()
                        P.mm(pB[:, 0:128], LF[:], Umat, True, True, [LF_b, cs_b], [pB_b])
                        P.mm(pB[:, 128:129], Umat, lfc, True, True, [cs_b, gtm_b], [pB_b])
                        return dict(j0=j0, ng=ng, j=j, jj=jj, latent=j0 >= 2, pB=(pB, pB_b), g=get_group(j0, ng))

                    def prepB(c):
                        j = c["j"]
                        latent = c["latent"]
                        pB, pB_b = c["pB"]
                        igc = gtm[:, j, gi:gi + 1]
                        Bs, Bs_b = Bsr.next()
                        P.cp("act", Bs[:], pB[:, 0:129], [pB_b], [Bs_b])
                        rc, rc_b = rcr.next()
                        P.tt("dve", rc[:, 0:1], igc, Bs[:, 128:129], ALU.subtract, [gtm_b, Bs_b], [rc_b])
                        Eb, Eb_b = Ebr.next()
                        P.act(Eb[:], Bs[:, 0:128], AF.Exp, [Bs_b], [Eb_b])
                        if latent:
                            DT, DT_b = DTr.next()
                            AT, AT_b = ATr.next()
                            P.stt("dve", DT[:], Bs[:, 0:128], rc[:, 0:1], maskT, ALU.add, ALU.add, [Bs_b, rc_b, cs_b], [DT_b])
                            c["AT"] = (AT, AT_b)
                        P.act(rc[:, 1:2], rc[:, 0:1], AF.Exp, [rc_b, Bs_b], [rc_b], bias=Bs[:, lc:lc + 1])
                        if latent:
                            P.act(AT[:], DT[:], AF.Exp, [DT_b], [AT_b])
                        c["rc"] = (rc, rc_b)
                        c["Eb"] = (Eb, Eb_b)

                    def prepC(c):
                        jj = c["jj"]
                        g = c["g"]
                        vt_, vt_b = g["vt"]
                        rc, rc_b = c["rc"]
                        Eb, Eb_b = c["Eb"]
                        vw, vw_b = vwr.next()
                        P.ts("dve", vw[:], vt_[:, jj, :], rc[:, 1:2], None, ALU.mult, None, [vt_b, rc_b], [vw_b])
                        c["vw"] = (vw, vw_b)
                        if c["latent"]:
                            kT_, kT_b = g["kT"]
                            qT_, qT_b = g["qT"]
                            AT, AT_b = c["AT"]
                            pG, pG_b = psr.next()
                            for cc in range(2):
                                P.mm(pG[:, 0:128], kT_[:, cc, jj * 128:(jj + 1) * 128], qT_[:, cc, jj * 128:(jj + 1) * 128], cc == 0, cc == 1,
                                     [kT_b, qT_b], [pG_b])
                            qs, qs_b = qsr.next()
                            for cc in range(2):
                                P.tt("dve", qs[:, cc, :], qT_[:, cc, jj * 128:(jj + 1) * 128], Eb[:], ALU.mult, [qT_b, Eb_b], [qs_b])
                            ST, ST_b = STr.next()
                            P.tt("dve", ST[:], pG[:, 0:128], AT[:], ALU.mult, [pG_b, AT_b], [ST_b])
                            c["ST"] = (ST, ST_b)
                            c["qs"] = (qs, qs_b)

                    def main(c):
                        j = c["j"]
                        jj = c["jj"]
                        tq = (j - 2) * 128
                        kt_, kt_b = c["g"]["kt"]
                        vt_, vt_b = c["g"]["vt"]
                        rc, rc_b = c["rc"]
                        Eb, Eb_b = c["Eb"]
                        vw, vw_b = c["vw"]
                        pUs = []
                        for cc in range(2):
                            pU, pU_b = psr.next()
                            P.mm(pU[:, 0:257], kt_[:, jj, cc * 128:(cc + 1) * 128], vw[:], True, True, [kt_b, vw_b], [pU_b])
                            pUs.append((pU, pU_b))
                        if c["latent"]:
                            ST, ST_b = c["ST"]
                            qs, qs_b = c["qs"]
                            pN, pN_b = psr.next()
                            P.mm(pN[:, 0:257], ST[:], vt_[:, jj, :], True, False, [ST_b, vt_b], [pN_b])
                            P.mm(pN[:, 0:257], qs[:, 0, :], CTb[:, 0, :], False, False, [qs_b, CTb_b], [pN_b])
                            P.mm(pN[:, 0:257], qs[:, 1, :], CTb[:, 1, :], False, True, [qs_b, CTb_b], [pN_b])
                        for cc in range(2):
                            pU, pU_b = pUs[cc]
                            P.stt("dve", CT[:, cc, :], CT[:, cc, :], Eb[:, lc:lc + 1], pU[:, 0:257], ALU.mult, ALU.add, [CT_b, Eb_b, pU_b], [CT_b])
                        P.cp("act", CTb[:], CT[:], [CT_b], [CTb_b])
                        if c["latent"]:
                            P.ts("dve", rc[:, 2:3], pN[:, 256:257], -1.0, None, ALU.mult, None, [pN_b], [rc_b])
                            P.tt("dve", rc[:, 3:4], rc[:, 2:3], pN[:, 256:257], ALU.max, [rc_b, pN_b], [rc_b])
                            P.ts("dve", rc[:, 3:4], rc[:, 3:4], 1.0, None, ALU.max, None, [rc_b], [rc_b])
                            P.op("dve", (lambda e, a=rc[:, 4:5], b=rc[:, 3:4]: e.reciprocal(out=a, in_=b)), [rc_b], [rc_b])
                            hch, hch_b = hchr.next()
                            P.ts("dve", hch[:], pN[:, 0:256], rc[:, 4:5], None, ALU.mult, None, [pN_b, rc_b], [hch_b])
                            if d_ == 0:
                                P.dma(dq.next(), HF[tq:tq + 128, hd * 256:(hd + 1) * 256], hch[:], reads=[hch_b], writes=[HF_b], lane_buf=hch_b)
                            else:
                                hfl, hfl_b = hflr.next()
                                P.dma(dq.next(), hfl[:], HF[tq:tq + 128, hd * 256:(hd + 1) * 256], reads=[HF_b], writes=[hfl_b], lane_buf=hfl_b)
                                hs_, hs_b = hsr.next()
                                P.tt("pool", hs_[:], hch[:], hfl[:], ALU.add, [hch_b, hfl_b], [hs_b])
                                P.memset("pool", rc[:, 5:6], 0.0, [rc_b])
                                c["hs"] = (hs_, hs_b)

                    def ro1(c):
                        if "hs" not in c:
                            return
                        rc, rc_b = c["rc"]
                        hs_, hs_b = c["hs"]
                        P.act(sqh[:], hs_[:], AF.Square, [hs_b, rc_b], [sqh_b, rc_b], accum_out=rc[:, 5:6])
                        P.act(rc[:, 6:7], rc[:, 5:6], AF.Ln, [rc_b], [rc_b], scale=1.0 / 256.0, bias=EPS)
                        P.act(rc[:, 7:8], rc[:, 6:7], AF.Exp, [rc_b], [rc_b], scale=-0.5)
                        hn, hn_b = hnr.next()
                        P.act(hn[:], hs_[:], AF.Copy, [hs_b, rc_b], [hn_b], scale=rc[:, 7:8])
                        c["hn"] = (hn, hn_b)

                    def ro2(c):
                        if "hn" not in c:
                            return
                        hn, hn_b = c["hn"]
                        pH, pH_b = psr.next()
                        for cc in range(2):
                            P.tr(pH[:, cc * 128:(cc + 1) * 128], hn[:, cc * 128:(cc + 1) * 128], ident, [hn_b, cs_b], [pH_b])
                        c["pH"] = (pH, pH_b)

                    def ro3(c):
                        if "pH" not in c:
                            return
                        tq = (c["j"] - 2) * 128
                        pH, pH_b = c["pH"]
                        hnT, hnT_b = hnTr.next()
                        P.cp("act", hnT[:].rearrange("p c t -> p (c t)"), pH[:, 0:256], [pH_b], [hnT_b])
                        P.dma(dq.next(), HNT[hd * 256:(hd + 1) * 256, tq:tq + 128].rearrange("(c p) t -> p c t", p=128), hnT[:],
                              reads=[hnT_b], writes=[HNT_b], lane_buf=hnT_b)

                    n_ = len(seq)
                    tiles = {}
                    for it in range(-3, n_ + 3):
                        if 0 <= it < n_:
                            main(tiles[it])
                        if 0 <= it - 1 < n_:
                            ro1(tiles[it - 1])
                        if 0 <= it - 2 < n_:
                            ro2(tiles[it - 2])
                        if 0 <= it - 3 < n_:
                            ro3(tiles.pop(it - 3))
                        if 0 <= it + 1 < n_:
                            prepC(tiles[it + 1])
                        if 0 <= it + 2 < n_:
                            prepB(tiles[it + 2])
                        if 0 <= it + 3 < n_:
                            tiles[it + 3] = prepA(*seq[it + 3])

        def stage_merge():
          with P.scope():
            stg = RR([P.sb([128, 8, 256], F32, f"mstg{i}") for i in range(2)])
            wout, wout_b = P.sb([128, 8, D], BF16, "wout")
            wr, wr_b = P.sb([128, 8, 16], F32, "wr")
            ldr = [RR([P.sb([128, 512], BF16 if n in (1, 2, 3) else F32, f"ld{n}{i}") for i in range(4)]) for n in range(6)]
            t1r = RR([P.sb([128, 512], F32, f"mt1{i}") for i in range(2)])
            t2r = RR([P.sb([128, 512], F32, f"mt2{i}") for i in range(2)])
            mTr = RR([P.sb([128, 8, 512], BF16, f"mT{i}") for i in range(2)])
            xtr = RR([P.sb([128, D], F32, f"mx{i}") for i in range(2)])
            x1r = RR([P.sb([128, D], F32, f"mx1{i}") for i in range(2)])
            tmpr = RR([P.sb([128, D], F32, f"mtmp{i}") for i in range(2)])
            h2r = RR([P.sb([128, D], F32, f"mh2{i}") for i in range(2)])
            h2br = RR([P.sb([128, D], BF16, f"mh2b{i}") for i in range(2)])
            sqm, sqm_b = P.sb([128, D], BF16, "sqm")
            ssr = RR([P.sb([128, 8], F32, f"mss{i}") for i in range(2)])
            h2Tr = RR([P.sb([128, 8, 128], F32, f"h2T{i}") for i in range(2)])
            lgr = RR([P.sb([128, 40], F32, f"lg{i}") for i in range(2)])
            dq = RR(["sp"])
            for j in range(4):
                st, st_b = stg.next()
                P.dma(dq.next(), st[:], w_out[:, j * 256:(j + 1) * 256].rearrange("(c p) n -> p c n", p=128), writes=[st_b], lane_buf=st_b)
                P.cp("act" if j % 2 else "dve", wout[:, :, j * 256:(j + 1) * 256], st[:], [st_b], [wout_b])
            P.dma("sp", wr[:], w_router, writes=[wr_b], lane_buf=wr_b)
            srcs = [(OT, OT_b), (GA, GA_b), (GM, GM_b), (ZS, ZS_b), (HNT, HNT_b), (XCT, XCT_b)]
            NB = int(os.environ.get("MRG_NB", "16"))
            def blk_gen(blk):
                tq0 = blk * 512
                mT, mT_b = mTr.next()
                for c in range(8):
                    lds = []
                    for n, (src, src_b) in enumerate(srcs):
                        t_, t_b = ldr[n].next()
                        P.dma(dq.next(), t_[:], src[c * 128:(c + 1) * 128, tq0:tq0 + 512], reads=[src_b], writes=[t_b], lane_buf=t_b)
                        lds.append((t_, t_b))
                    (ot, ot_b), (ga, ga_b), (gm_, gm_b), (zs, zs_b), (hnt, hnt_b), (xct, xct_b) = lds
                    t1, t1_b = t1r.next()
                    t2, t2_b = t2r.next()
                    P.act(t1[:], xct[:], AF.Copy, [xct_b, colt_b], [t1_b], scale=colt[:, c, 3:4])
                    P.stt("dve", t2[:], hnt[:], colt[:, c, 2:3], t1[:], ALU.mult, ALU.add, [hnt_b, colt_b, t1_b], [t2_b])
                    P.tt("pool", t1[:], t2[:], zs[:], ALU.mult, [t2_b, zs_b], [t1_b])
                    P.tt("dve", t2[:], ot[:], ga[:], ALU.mult, [ot_b, ga_b], [t2_b])
                    P.tt("pool", t1[:], t1[:], gm_[:], ALU.mult, [t1_b, gm_b], [t1_b])
                    P.tt("dve", mT[:, c, :], t2[:], t1[:], ALU.add, [t2_b, t1_b], [mT_b])
                yield
                for i in range(4):
                    tok = tq0 + i * 128
                    jt = tok // 128
                    xt, xt_b = xtr.next()
                    P.dma(dq.next(), xt[:], xcat[TC + tok:TC + tok + 128, :], writes=[xt_b], lane_buf=xt_b)
                    tmp, tmp_b = tmpr.next()
                    for hh in range(2):
                        pt, pt_b = psr.next()
                        for k in range(8):
                            P.mm(pt[:, :], mT[:, k, i * 128:(i + 1) * 128], wout[:, k, hh * 512:(hh + 1) * 512], k == 0, k == 7, [mT_b, wout_b], [pt_b])
                        P.tt("dve", tmp[:, hh * 512:(hh + 1) * 512], pt[:, :], bc[:, 0, hh * 512:(hh + 1) * 512], ALU.mult, [pt_b, bc_b], [tmp_b])
                    x1t, x1t_b = x1r.next()
                    P.tt("dve", x1t[:], tmp[:], xt[:], ALU.add, [tmp_b, xt_b], [x1t_b])
                    P.dma(dq.next(), X1[tok:tok + 128, :], x1t[:], reads=[x1t_b], writes=[X1_b], lane_buf=x1t_b)
                    ss, ss_b = ssr.next()
                    P.memset("dve", ss[:], 0.0, [ss_b])
                    P.act(sqm[:], x1t[:], AF.Square, [x1t_b, ss_b], [sqm_b, ss_b], accum_out=ss[:, 0:1])
                    P.act(ss[:, 1:2], ss[:, 0:1], AF.Ln, [ss_b], [ss_b], scale=1.0 / D, bias=EPS)
                    P.act(ss[:, 2:3], ss[:, 1:2], AF.Exp, [ss_b], [ss_b], scale=-0.5)
                    h2, h2_b = h2r.next()
                    P.stt("dve", h2[:], x1t[:], ss[:, 2:3], bc[:, 1, :], ALU.mult, ALU.mult, [x1t_b, ss_b, bc_b], [h2_b])
                    P.tt("pool", h2[:], h2[:], bc[:, 2, :], ALU.add, [h2_b, bc_b], [h2_b])
                    h2b, h2b_b = h2br.next()
                    P.cp("act", h2b[:], h2[:], [h2_b], [h2b_b])
                    P.dma(dq.next(), H2[tok:tok + 128, :], h2b[:], reads=[h2b_b], writes=[H2_b], lane_buf=h2b_b)
                    h2T, h2T_b = h2Tr.next()
                    for hh in range(2):
                        pt, pt_b = psr.next()
                        for c4 in range(4):
                            c = hh * 4 + c4
                            P.tr(pt[:, c4 * 128:(c4 + 1) * 128], h2[:, c * 128:(c + 1) * 128], ident, [h2_b, cs_b], [pt_b])
                        P.cp("act" if hh else "dve", h2T[:, hh * 4:(hh + 1) * 4, :].rearrange("p c t -> p (c t)"), pt[:, :], [pt_b], [h2T_b])
                    pt, pt_b = psr.next()
                    for c in range(8):
                        P.mm(pt[:, 0:16], h2T[:, c, :], wr[:, c, :], c == 0, c == 7, [h2T_b, wr_b], [pt_b])
                    lg, lg_b = lgr.next()
                    P.cp("dve", lg[:, 0:16], pt[:, 0:16], [pt_b], [lg_b])
                    P.op("dve", (lambda e, a=lg[:, 32:33], b=lg[:, 0:16]: e.tensor_reduce(out=a, in_=b, axis=AX.X, op=ALU.max)), [lg_b], [lg_b])
                    P.ts("dve", lg[:, 33:34], lg[:, 32:33], -1.0, None, ALU.mult, None, [lg_b], [lg_b])
                    P.memset("dve", lg[:, 34:35], 0.0, [lg_b])
                    P.act(lg[:, 16:32], lg[:, 0:16], AF.Exp, [lg_b], [lg_b], bias=lg[:, 33:34], accum_out=lg[:, 34:35])
                    P.op("dve", (lambda e, a=lg[:, 35:36], b=lg[:, 34:35]: e.reciprocal(out=a, in_=b)), [lg_b], [lg_b])
                    P.ts("dve", aff[:, jt, :], lg[:, 16:32], lg[:, 35:36], None, ALU.mult, None, [lg_b], [aff_b])
            gens = [blk_gen(b_) for b_ in range(NB)]
            if gens:
                next(gens[0])
            for gi_ in range(len(gens)):
                if gi_ + 1 < len(gens):
                    next(gens[gi_ + 1])
                for _ in gens[gi_]:
                    pass
            if "DBG_aff" in P.debug:
                da_, da_b = P.dram("DBG_aff", [128, 64 * 16], F32)
                finals.append(P.dma("sp", da_, aff[:].rearrange("p j g -> p (j g)"), reads=[aff_b], lane_buf=aff_b))

        def stage_route():
          with P.scope():
            lo, lo_b = P.sb([128, 16], F32, "lo")
            hi, hi_b = P.sb([128, 16], F32, "hi")
            mid, mid_b = P.sb([128, 16], F32, "mid")
            cntp, cntp_b = P.sb([128, 16], F32, "cntp")
            ge, ge_b = P.sb([128, 16], F32, "ge")
            ta, ta_b = P.sb([128, 16], F32, "ta")
            tb, tb_b = P.sb([128, 16], F32, "tb")
            eoff, eoff_b = P.sb([128, 16], F32, "eoff")
            cmpt, cmpt_b = P.sb([128, 64, 16], F32, "cmp")
            maskf, maskf_b = P.sb([128, 64, 16], F32, "maskf")
            maskb, maskb_b = P.sb([128, 1024], BF16, "maskb")
            pwS, pwS_b = P.sb([128, 64, 16], F32, "pwS")
            totS, totS_b = P.sb([128, 64, 16], F32, "totS")
            scA, scA_b = P.sb([128, 64, 16], F32, "scA")
            scB, scB_b = P.sb([128, 64, 16], F32, "scB")
            onesf = cs[:, 7, :]
            P.memset("dve", lo[:], 0.0, [lo_b])
            P.memset("dve", hi[:], 2.0, [hi_b])
            for e_ in range(16):
                P.memset("pool", eoff[:, e_:e_ + 1], float(e_ * CAP), [eoff_b])
            affv = aff[:]
            for it in range(int(os.environ.get("ROUTE_ITERS", "34"))):
                P.tt("dve", mid[:], lo[:], hi[:], ALU.add, [lo_b, hi_b], [mid_b])
                P.ts("dve", mid[:], mid[:], 0.5, None, ALU.mult, None, [mid_b], [mid_b])
                P.tt("dve", cmpt[:], affv, mid[:].unsqueeze(1).to_broadcast([128, 64, 16]), ALU.is_ge, [aff_b, mid_b], [cmpt_b])
                P.op("dve", (lambda e, a=cntp[:], b=cmpt[:].rearrange("p j g -> p g j"): e.tensor_reduce(out=a, in_=b, axis=AX.X, op=ALU.add)),
                     [cmpt_b], [cntp_b])
                pt, pt_b = psr.next()
                P.mm(pt[:, 0:16], onesf, cntp[:], True, True, [cs_b, cntp_b], [pt_b])
                P.ts("dve", ge[:], pt[:, 0:16], float(CAP), None, ALU.is_ge, None, [pt_b], [ge_b])
                P.tt("dve", ta[:], ge[:], mid[:], ALU.mult, [ge_b, mid_b], [ta_b])
                P.tt("dve", lo[:], lo[:], ta[:], ALU.max, [lo_b, ta_b], [lo_b])
                P.stt("dve", tb[:], ge[:], 4.0, mid[:], ALU.mult, ALU.add, [ge_b, mid_b], [tb_b])
                P.tt("dve", hi[:], hi[:], tb[:], ALU.min, [hi_b, tb_b], [hi_b])
            P.tt("dve", maskf[:], affv, lo[:].unsqueeze(1).to_broadcast([128, 64, 16]), ALU.is_ge, [aff_b, lo_b], [maskf_b])
            P.cp("dve", maskb[:], maskf[:].rearrange("p j g -> p (j g)"), [maskf_b], [maskb_b])
            for hh in range(2):
                pt, pt_b = psr.next()
                P.mm(pt[:, :], csb[:, 5, :], maskb[:, hh * 512:(hh + 1) * 512], True, True, [csb_b, maskb_b], [pt_b])
                P.cp("dve", pwS[:].rearrange("p j g -> p (j g)")[:, hh * 512:(hh + 1) * 512], pt[:, :], [pt_b], [pwS_b])
                pt, pt_b = psr.next()
                P.mm(pt[:, :], onesb, maskb[:, hh * 512:(hh + 1) * 512], True, True, [csb_b, maskb_b], [pt_b])
                P.cp("act", totS[:].rearrange("p j g -> p (j g)")[:, hh * 512:(hh + 1) * 512], pt[:, :], [pt_b], [totS_b])
            P.cp("dve", scA[:], totS[:], [totS_b], [scA_b])
            A, A_b, B, B_b = scA, scA_b, scB, scB_b
            for sft in (1, 2, 4, 8, 16, 32):
                P.cp("dve", B[:, 0:sft, :], A[:, 0:sft, :], [A_b], [B_b])
                P.tt("dve", B[:, sft:64, :], A[:, sft:64, :], A[:, 0:64 - sft, :], ALU.add, [A_b], [B_b])
                A, A_b, B, B_b = B, B_b, A, A_b
            P.tt("dve", B[:], A[:], totS[:], ALU.subtract, [A_b, totS_b], [B_b])
            P.tt("dve", B[:], B[:], pwS[:], ALU.add, [B_b, pwS_b], [B_b])
            P.ts("dve", A[:], B[:], float(CAP), None, ALU.is_lt, None, [B_b], [A_b])
            P.tt("dve", A[:], A[:], maskf[:], ALU.mult, [A_b, maskf_b], [A_b])
            P.tt("dve", gmv[:], affv, A[:], ALU.mult, [aff_b, A_b], [gmv_b])
            P.tt("dve", B[:], B[:], eoff[:].unsqueeze(1).to_broadcast([128, 64, 16]), ALU.add, [B_b, eoff_b], [B_b])
            P.stt("dve", B[:], B[:], -1.0e6, A[:], ALU.add, ALU.mult, [B_b, A_b], [B_b])
            P.ts("dve", B[:], B[:], 1.0e6, None, ALU.add, None, [B_b], [B_b])
            P.cp("dve", posi[:], B[:].rearrange("p j g -> p (j g)"), [B_b], [posi_b])
            if "DBG_pos" in P.debug:
                dp_, dp_b = P.dram("DBG_pos", [128, 64 * 16], I32)
                finals.append(P.dma("sp", dp_, posi[:], reads=[posi_b], lane_buf=posi_b))
                dg2_, dg2_b = P.dram("DBG_gmv", [128, 64 * 16], F32)
                finals.append(P.dma("sp", dg2_, gmv[:].rearrange("p j g -> p (j g)"), reads=[gmv_b], lane_buf=gmv_b))

        def stage_scatter():
          with P.scope():
            h2lr = RR([P.sb([128, D], BF16, f"h2l{i}") for i in range(3)])
            for j in range(64):
                h2l, h2l_b = h2lr.next()
                P.dma("sp", h2l[:], H2[j * 128:(j + 1) * 128, :], reads=[H2_b], writes=[h2l_b], lane_buf=h2l_b)
                for e_ in range(NEXP):
                    P.dma("pool", None, None, reads=[h2l_b, posi_b], lane_buf=h2l_b,
                          fn=(lambda e, o=XS[:, :], off=posi[:, j * 16 + e_:j * 16 + e_ + 1], i_=h2l[:, :]: e.indirect_dma_start(
                              out=o, out_offset=bass.IndirectOffsetOnAxis(ap=off, axis=0), in_=i_, in_offset=None,
                              bounds_check=P.pool_reg, oob_is_err=False)))

        def stage_experts():
          with P.scope():
            identb = csb[:, 0, :]
            xsr = RR([P.sb([128, D], BF16, f"xs{i}") for i in range(3)])
            xsT, xsT_b = P.sb([128, 8, CAP], BF16, "xsT")
            wtr = [RR([P.sb([128, 8, D], BF16, f"we{i}_{k}") for k in range(2)]) for i in range(3)]
            actT, actT_b = P.sb([128, 8, CAP], BF16, "actT")
            sar = RR([P.sb([128, 512], F32, f"sa{i}") for i in range(2)])
            ysr = RR([P.sb([128, D], F32, f"ys{i}") for i in range(2)])
            NE_ = int(os.environ.get("MOE_NE", "16"))

            def load_w(e_):
                res = []
                for wi, wsrc in enumerate((w_eg, w_eu, w_ed)):
                    wt, wt_b = wtr[wi].next()
                    for j in range(4):
                        P.dma("pool", wt[:, :, j * 256:(j + 1) * 256], wsrc[e_, :, j * 256:(j + 1) * 256].rearrange("(c p) n -> p c n", p=128),
                              writes=[wt_b], lane_buf=wt_b)
                    res.append((wt, wt_b))
                return res
            wnext = load_w(0)
            for e_ in range(NE_):
                (wg_, wg_b), (wu_, wu_b), (wd_, wd_b) = wnext
                if e_ + 1 < NE_:
                    wnext = load_w(e_ + 1)
                for i in range(8):
                    xs, xs_b = xsr.next()
                    P.dma("sp", xs[:], XS[e_ * CAP + i * 128:e_ * CAP + (i + 1) * 128, :], reads=[XS_b], writes=[xs_b], lane_buf=xs_b)
                    for k in range(8):
                        P.tr(psbf[:, k * 128:(k + 1) * 128], xs[:, k * 128:(k + 1) * 128], identb, [xs_b, csb_b], [psbf_b])
                    P.cp("act" if i % 2 else "dve", xsT[:, :, i * 128:(i + 1) * 128], psbf[:, :].rearrange("p (k t) -> p k t", t=128), [psbf_b], [xsT_b])
                for f in range(8):
                    for hh in range(2):
                        pa, pa_b = psr.next()
                        pu, pu_b = psr.next()
                        for k in range(8):
                            P.mm(pa[:, :], wg_[:, k, f * 128:(f + 1) * 128], xsT[:, k, hh * 512:(hh + 1) * 512], k == 0, k == 7, [wg_b, xsT_b], [pa_b])
                        for k in range(8):
                            P.mm(pu[:, :], wu_[:, k, f * 128:(f + 1) * 128], xsT[:, k, hh * 512:(hh + 1) * 512], k == 0, k == 7, [wu_b, xsT_b], [pu_b])
                        sa, sa_b = sar.next()
                        P.act(sa[:], pa[:, :], AF.Silu, [pa_b], [sa_b])
                        P.tt("dve", actT[:, f, hh * 512:(hh + 1) * 512], sa[:], pu[:, :], ALU.mult, [sa_b, pu_b], [actT_b])
                for i in range(8):
                    ys, ys_b = ysr.next()
                    for hh in range(2):
                        py, py_b = psr.next()
                        for f in range(8):
                            P.mm(py[:, :], actT[:, f, i * 128:(i + 1) * 128], wd_[:, f, hh * 512:(hh + 1) * 512], f == 0, f == 7, [actT_b, wd_b], [py_b])
                        P.tt("dve", ys[:, hh * 512:(hh + 1) * 512], py[:, :], bc[:, 3, hh * 512:(hh + 1) * 512], ALU.mult, [py_b, bc_b], [ys_b])
                    P.dma("sp", YY[e_ * CAP + i * 128:e_ * CAP + (i + 1) * 128, :], ys[:], reads=[ys_b], lane_buf=ys_b)

        def stage_final():
          with P.scope():
            ygr = RR([P.sb([128, D], F32, f"yg{i}") for i in range(8)])
            x1r = RR([P.sb([128, D], F32, f"fx1{i}") for i in range(2)])
            acr = RR([P.sb([128, D], F32, f"fac{i}") for i in range(2)])
            tmr = RR([P.sb([128, D], F32, f"ftm{i}") for i in range(2)])
            outr = RR([P.sb([128, D], F32, f"fo{i}") for i in range(2)])
            sqf, sqf_b = P.sb([128, D], BF16, "sqf")
            ssr = RR([P.sb([128, 8], F32, f"fss{i}") for i in range(2)])
            for (yg_, yg_b) in ygr.items:
                P.memset("pool", yg_[:], 0.0, [yg_b])
            NJ = int(os.environ.get("FIN_NJ", "64"))
            for j in range(NJ):
                x1t, x1t_b = x1r.next()
                P.dma("sp", x1t[:], X1[j * 128:(j + 1) * 128, :], reads=[X1_b], writes=[x1t_b], lane_buf=x1t_b)
                tm, tm_b = tmr.next()
                for e_ in range(NEXP):
                    yg_, yg_b = ygr.next()
                    P.dma("pool", None, None, reads=[posi_b], writes=[yg_b], lane_buf=yg_b,
                          fn=(lambda e, o=yg_[:, :], off=posi[:, j * 16 + e_:j * 16 + e_ + 1], i_=YY[:, :]: e.indirect_dma_start(
                              out=o, out_offset=None, in_=i_, in_offset=bass.IndirectOffsetOnAxis(ap=off, axis=0),
                              bounds_check=P.pool_reg, oob_is_err=False)))
                    if e_ == 0:
                        P.stt("dve", tm[:], yg_[:], gmv[:, j, e_:e_ + 1], x1t[:], ALU.mult, ALU.add, [yg_b, gmv_b, x1t_b], [tm_b])
                    else:
                        P.stt("dve", tm[:], yg_[:], gmv[:, j, e_:e_ + 1], tm[:], ALU.mult, ALU.add, [yg_b, gmv_b, tm_b], [tm_b])
                ss, ss_b = ssr.next()
                P.memset("dve", ss[:], 0.0, [ss_b])
                P.act(sqf[:], tm[:], AF.Square, [tm_b, ss_b], [sqf_b, ss_b], accum_out=ss[:, 0:1])
                P.act(ss[:, 1:2], ss[:, 0:1], AF.Ln, [ss_b], [ss_b], scale=1.0 / D, bias=EPS)
                P.act(ss[:, 2:3], ss[:, 1:2], AF.Exp, [ss_b], [ss_b], scale=-0.5)
                ot_, ot_b = outr.next()
                P.stt("dve", ot_[:], tm[:], ss[:, 2:3], bc[:, 4, :], ALU.mult, ALU.mult, [tm_b, ss_b, bc_b], [ot_b])
                finals.append(P.dma("sp", out[j * 128:(j + 1) * 128, :], ot_[:], reads=[ot_b], lane_buf=ot_b))

        if "2" in ST:
            stage2()
        if "attn" in ST:
            stage_attn()
        if "scan" in ST:
            stage_scan()
        if "merge" in ST:
            stage_merge()
        if "moe" in ST:
            stage_route()
            stage_scatter()
            stage_experts()
        if "final" in ST:
            stage_final()

        for nm, (ap_, b_) in {"XMT": (XMT, XMT_b), "ZS": (ZS, ZS_b), "GA": (GA, GA_b), "GM": (GM, GM_b), "QN": (QN, QN_b),
                              "QR": (QR, QR_b), "KN": (KN, KN_b), "KR": (KR, KR_b), "VV": (VV, VV_b)}.items():
            if nm in P.debug and b_.last_w is not None:
                pass
        last = P.barrier()
        finals.append(last)
        P.emit(final_wait_ops=finals)
    return nc, P


def _chunks(v, n):
    return np.ascontiguousarray(np.asarray(v, np.float32).reshape(n, 128).T)


def prep_inputs(x, c, ctx, c_ctx, w_mod, b_mod, norm1, w_in, q_norm, w_uq, kv_norm, w_ukv, conv_w, conv_b,
                w_qblk, w_kblk, w_vblk, w_gate, b_gate, ml_norm, ml_skip, w_out, norm2, w_router,
                w_e_gate, w_e_up, w_e_down, final_norm):
    f = lambda a: np.asarray(a, np.float32)
    shared = {}
    shared["w_mod"] = np.ascontiguousarray(f(w_mod)[0])
    rowv = np.zeros((2, 8 * D), np.float32)
    rowv[0, :6 * D] = f(b_mod)[0]
    rowv[1, :6 * D] = f(b_mod)[0]
    rowv[0, 6 * D:7 * D] = f(norm2)[0]
    rowv[0, 7 * D:8 * D] = f(final_norm)
    shared["rowv"] = rowv
    colv = np.zeros((128, 8, 9), np.float32)
    colv[:, :, 0] = _chunks(f(norm1)[0], 8)
    colv[:, :, 1] = _chunks(f(conv_b)[0], 8)
    colv[:, :, 2] = _chunks(f(ml_norm)[0], 8)
    colv[:, :, 3] = _chunks(f(ml_skip)[0], 8)
    for j in range(5):
        colv[:, :, 4 + j] = _chunks(f(conv_w)[0, j], 8)
    shared["colv"] = colv
    qkn = np.zeros((128, 5), np.float32)
    qkn[:, 0:3] = _chunks(f(q_norm)[0], 3)
    qkn[:, 3:5] = _chunks(f(kv_norm)[0], 2)
    shared["qkn"] = qkn
    perm = np.concatenate([np.arange(16, 32), np.arange(0, 16), np.arange(48, 64), np.arange(32, 48)])
    wi = f(w_in)[0]
    shared["w_in"] = np.ascontiguousarray(np.concatenate([wi, wi[:, 640:704][:, perm]], axis=1))
    wq = f(w_uq)[0].reshape(384, 8, 192)
    shared["w_uq"] = np.ascontiguousarray(np.concatenate(
        [wq[:, :, :128].reshape(384, 1024), wq[:, :, 128:].reshape(384, 512), wq[:, :, 128:][:, :, perm].reshape(384, 512)], axis=1))
    wkv = f(w_ukv)[0].reshape(256, 8, 256)
    shared["w_ukv"] = np.ascontiguousarray(np.concatenate([wkv[:, :, :128].reshape(256, 1024), wkv[:, :, 128:].reshape(256, 1024)], axis=1))
    pos = np.arange(T)
    row = (pos // 64).astype(np.float32)
    col = (pos % 64).astype(np.float32)
    inv = (np.float32(10000.0) ** (-np.arange(16, dtype=np.float32) / np.float32(16))).astype(np.float32)
    ar = row[:, None] * inv
    ac = col[:, None] * inv
    cos64 = np.concatenate([np.cos(ar), np.cos(ar), np.cos(ac), np.cos(ac)], axis=1).T
    sin64 = np.concatenate([-np.sin(ar), np.sin(ar), -np.sin(ac), np.sin(ac)], axis=1).T
    ropet = np.zeros((2, 128, TA), np.float32)
    ropet[0, :, :TC] = 1.0
    ropet[0, 0:64, TC:] = cos64
    ropet[0, 64:128, TC:] = cos64
    ropet[1, 0:64, TC:] = sin64
    ropet[1, 64:128, TC:] = sin64
    shared["ropet"] = ropet
    cst = np.zeros((128, 8, 128), np.float32)
    i = np.arange(128)
    cst[:, 0, :] = np.eye(128)
    cst[:, 1, :] = (i[:, None] <= i[None, :])
    cst[:, 2, :] = (i[:, None] >= i[None, :])
    cst[:, 3, :] = np.where(i[:, None] <= i[None, :], 0.0, NEG)
    cst[:, 4, :] = np.where(i[:, None] >= i[None, :], 0.0, NEG)
    cst[:, 5, :] = (i[:, None] < i[None, :])
    cst[0, 6, :] = 1.0
    cst[:, 7, :] = 1.0
    shared["cst"] = cst
    wbd = np.zeros((128, 24, 128), np.float32)
    for wi_, wsrc in enumerate((w_qblk, w_kblk, w_vblk)):
        wb = f(wsrc)[0]
        for cc in range(8):
            for nn in range(32):
                wbd[nn * 4:(nn + 1) * 4, wi_ * 8 + cc, nn * 4:(nn + 1) * 4] = wb[cc * 32 + nn]
    shared["w_bd"] = wbd
    shared["w_gate"] = np.ascontiguousarray(f(w_gate)[0].reshape(24, 128, 16).transpose(1, 0, 2))
    shared["b_gate"] = np.ascontiguousarray(f(b_gate)[0].reshape(16, 1))
    shared["w_out"] = np.ascontiguousarray(f(w_out)[0])
    shared["w_router"] = np.ascontiguousarray(f(w_router)[0].reshape(8, 128, 16).transpose(1, 0, 2))
    shared["w_eg"] = np.ascontiguousarray(f(w_e_gate)[0])
    shared["w_eu"] = np.ascontiguousarray(f(w_e_up)[0])
    shared["w_ed"] = np.ascontiguousarray(f(w_e_down)[0])
    shared["tokid"] = np.ascontiguousarray((np.arange(64)[None, :] * 128 + np.arange(128)[:, None]).astype(np.int32))
    per_b = []
    for b in range(2):
        d = dict(shared)
        d["xcat"] = np.ascontiguousarray(np.concatenate([f(ctx)[b], f(x)[b]], axis=0))
        cv = np.zeros((128, 8, 2), np.float32)
        cv[:, :, 0] = _chunks(f(c)[b], 8)
        cv[:, :, 1] = _chunks(f(c_ctx), 8)
        d["cvec"] = cv
        per_b.append(d)
    return per_b


def kernel(**inputs):
    per_b = prep_inputs(**inputs)
    nc, P = build()
    in_maps = [per_b[0], per_b[1]]
    res = run_bass_kernel_spmd(nc, in_maps, core_ids=[0, 1])
    return np.stack([res.results[0]["out"], res.results[1]["out"]], axis=0).astype(np.float32)
```
